# Optimizing a Trainium2 kernel written in Bass

```python
import jax, jax.numpy as jnp
from jax import lax
import numpy as np

D_MODEL = 1024
BATCH = 16
SEQ = 2048
DEPTH = 1

N_HEADS = 8
N_KV_HEADS = 2
HEAD_DIM = 64
Q_WIDTH = N_HEADS * HEAD_DIM
KV_WIDTH = N_KV_HEADS * HEAD_DIM
ROPE_THETA = 500000.0
ROT_FRACTION = 4
IDX_HEADS = 8
IDX_DIM = 64
IDXQ_WIDTH = IDX_HEADS * IDX_DIM
TOPK_MAX = 256
Q_BLOCK = 128
CONV_DIM = 512
CONV_WIDTH = 3
N_BRANCHES = 2
IN_SPLIT_SIZES = (Q_WIDTH, KV_WIDTH, KV_WIDTH, IDXQ_WIDTH, IDX_DIM, IDX_HEADS,
                  CONV_DIM, CONV_DIM, CONV_DIM, N_BRANCHES * D_MODEL)
IN_WIDTH = Q_WIDTH + 2 * KV_WIDTH + IDXQ_WIDTH + IDX_DIM + IDX_HEADS + 3 * CONV_DIM + N_BRANCHES * D_MODEL
N_GROUPS = 4
EXPERTS_PER_GROUP = 8
N_EXPERTS = N_GROUPS * EXPERTS_PER_GROUP
EXPERT_TOPK = 2
D_EXPERT = 512
EXPERT_BLOCK = 128
LN_EPS = 1e-5
DEEPNORM_ALPHA = (2 * DEPTH) ** 0.25
DEEPNORM_BETA = (8 * DEPTH) ** -0.25

kernel_name = "hybrid_dsa_shortconv_hmoe_block"


def _split_points():
    return [int(s) for s in np.cumsum(IN_SPLIT_SIZES)[:-1]]


def layer_norm(x, g, b):
    xf = x.astype(jnp.float32)
    mu = jnp.mean(xf, axis=-1, keepdims=True)
    var = jnp.mean(jnp.square(xf - mu), axis=-1, keepdims=True)
    y = (xf - mu) * lax.rsqrt(var + LN_EPS)
    return (y * g.astype(jnp.float32) + b.astype(jnp.float32)).astype(x.dtype)


def partial_rope(t, pos):
    rot = t.shape[-1] // ROT_FRACTION
    half = rot // 2
    inv_freq = ROPE_THETA ** (-jnp.arange(half, dtype=jnp.float32) / half)
    ang = pos.astype(jnp.float32)[:, None] * inv_freq[None, :]
    cos = jnp.cos(ang)[None, :, None, :].astype(t.dtype)
    sin = jnp.sin(ang)[None, :, None, :].astype(t.dtype)
    x1 = t[..., :half]
    x2 = t[..., half:rot]
    return jnp.concatenate([x1 * cos - x2 * sin, x2 * cos + x1 * sin, t[..., rot:]], axis=-1)


def dsa_attention(q, k, v, qi, ki, wi, top_k):
    B, S = q.shape[0], q.shape[1]
    n_blocks = S // Q_BLOCK
    group = N_HEADS // N_KV_HEADS
    kpos = jnp.arange(S)

    def to_blocks(t):
        return jnp.moveaxis(t.reshape((B, n_blocks, Q_BLOCK) + t.shape[2:]), 1, 0)

    def one_block(args):
        qb, qib, wib, t0 = args
        qpos = t0 + jnp.arange(Q_BLOCK)
        admissible = kpos[None, :] <= qpos[:, None]
        rel = jax.nn.relu(jnp.einsum('bqhd,bsd->bqhs', qib, ki) * (IDX_DIM ** -0.5))
        score = jnp.einsum('bqhs,bqh->bqs', rel, wib) * (IDX_HEADS ** -0.5)
        score = jnp.where(admissible[None], score.astype(jnp.float32), -jnp.inf)
        _, idx = lax.top_k(score, top_k)
        valid = idx <= qpos[None, :, None]
        kg = jax.vmap(lambda kb, ib: kb[ib])(k, idx)
        vg = jax.vmap(lambda vb, ib: vb[ib])(v, idx)
        qg = qb.reshape(B, Q_BLOCK, N_KV_HEADS, group, HEAD_DIM)
        logits = jnp.einsum('bqhgd,bqkhd->bqhgk', qg, kg).astype(jnp.float32) * (HEAD_DIM ** -0.5)
        logits = jnp.where(valid[:, :, None, None, :], logits, -jnp.inf)
        p = jax.nn.softmax(logits, axis=-1).astype(vg.dtype)
        o = jnp.einsum('bqhgk,bqkhd->bqhgd', p, vg)
        return o.reshape(B, Q_BLOCK, N_HEADS * HEAD_DIM)

    t0s = jnp.arange(n_blocks) * Q_BLOCK
    out = lax.map(one_block, (to_blocks(q), to_blocks(qi), to_blocks(wi), t0s))
    return jnp.moveaxis(out, 0, 1).reshape(B, S, Q_WIDTH)


def short_conv(conv_in, conv_b, conv_c, conv_w):
    u = conv_c * conv_in
    rhs = conv_w[:, None, :].astype(u.dtype)
    y = lax.conv_general_dilated(u, rhs, window_strides=(1,), padding=[(CONV_WIDTH - 1, 0)],
                                 dimension_numbers=('NWC', 'WIO', 'NWC'),
                                 feature_group_count=CONV_DIM)
    return conv_b * y


def token_mixer(h, pos, top_k, w_in, gate_bias, w_attn_up, w_conv_up, conv_w, w_out):
    B, S, _ = h.shape
    proj = h @ w_in
    q, k, v, qi, ki, wi, conv_in, conv_b, conv_c, gates = jnp.split(proj, _split_points(), axis=-1)
    q = partial_rope(q.reshape(B, S, N_HEADS, HEAD_DIM), pos)
    k = partial_rope(k.reshape(B, S, N_KV_HEADS, HEAD_DIM), pos)
    v = v.reshape(B, S, N_KV_HEADS, HEAD_DIM)
    qi = partial_rope(qi.reshape(B, S, IDX_HEADS, IDX_DIM), pos)
    ki = partial_rope(ki.reshape(B, S, 1, IDX_DIM), pos)[:, :, 0]
    attn = dsa_attention(q, k, v, qi, ki, wi, top_k)
    conv = short_conv(conv_in, conv_b, conv_c, conv_w)
    g_attn, g_conv = jnp.split(jax.nn.sigmoid(gates + gate_bias), N_BRANCHES, axis=-1)
    merged = g_attn * (attn @ w_attn_up) + g_conv * (conv @ w_conv_up)
    return merged @ w_out


def hierarchical_moe(h, rg_w, rg_b, re_w, re_b, w_gate, w_up, w_down):
    B, S, D = h.shape
    n_tok = B * S
    xf = h.reshape(n_tok, D)
    g_logits = (xf @ rg_w + rg_b).astype(jnp.float32)
    grp = jnp.argmax(g_logits, axis=-1).astype(jnp.int32)
    p_grp = jnp.take_along_axis(jax.nn.softmax(g_logits, axis=-1), grp[:, None], axis=1)[:, 0]
    e_all = jnp.einsum('nd,gde->nge', xf, re_w) + re_b
    e_logits = jnp.take_along_axis(e_all, grp[:, None, None], axis=1)[:, 0].astype(jnp.float32)
    top_val, top_loc = lax.top_k(e_logits, EXPERT_TOPK)
    gate = p_grp[:, None] * jax.nn.softmax(top_val, axis=-1)
    expert = grp[:, None] * EXPERTS_PER_GROUP + top_loc.astype(jnp.int32)
    n_assign = n_tok * EXPERT_TOPK
    a_expert = expert.reshape(-1)
    a_token = jnp.repeat(jnp.arange(n_tok, dtype=jnp.int32), EXPERT_TOPK)
    a_w = gate.reshape(-1).astype(h.dtype)
    order = jnp.argsort(a_expert, stable=True)
    se, st, sw = a_expert[order], a_token[order], a_w[order]
    counts = jax.ops.segment_sum(jnp.ones_like(a_expert), a_expert, num_segments=N_EXPERTS)
    padded = ((counts + EXPERT_BLOCK - 1) // EXPERT_BLOCK) * EXPERT_BLOCK
    pad_end = jnp.cumsum(padded)
    pad_start = pad_end - padded
    raw_start = jnp.cumsum(counts) - counts
    dest = pad_start[se] + jnp.arange(n_assign, dtype=jnp.int32) - raw_start[se]
    cap = (-(-n_assign // EXPERT_BLOCK)) * EXPERT_BLOCK + N_EXPERTS * EXPERT_BLOCK
    n_blocks = cap // EXPERT_BLOCK
    row_token = jnp.zeros((cap,), jnp.int32).at[dest].set(st)
    row_w = jnp.zeros((cap,), h.dtype).at[dest].set(sw)
    block_start = jnp.arange(n_blocks, dtype=jnp.int32) * EXPERT_BLOCK
    block_expert = jnp.minimum(jnp.searchsorted(pad_end, block_start, side='right'), N_EXPERTS - 1)
    xs = xf[row_token].reshape(n_blocks, EXPERT_BLOCK, D)

    def expert_block(args):
        xb, e = args
        hid = jax.nn.silu(xb @ w_gate[e]) * (xb @ w_up[e])
        return hid @ w_down[e]

    ys = lax.map(expert_block, (xs, block_expert)).reshape(cap, D)
    out = jax.ops.segment_sum(ys * row_w[:, None], row_token, num_segments=n_tok)
    return out.reshape(B, S, D)


def setup_inputs(seed: int = 0) -> dict:
    key = jax.random.key(seed)
    ks = jax.random.split(key, 20)
    nrm = jax.random.normal
    L = DEPTH
    v_lo = Q_WIDTH + KV_WIDTH
    v_hi = v_lo + KV_WIDTH
    w_in = nrm(ks[1], (L, D_MODEL, IN_WIDTH), jnp.float32) * D_MODEL ** -0.5
    w_in = w_in.at[:, :, v_lo:v_hi].multiply(DEEPNORM_BETA)
    return {
        "x": nrm(ks[0], (BATCH, SEQ, D_MODEL), jnp.float32),
        "w_in": w_in,
        "gate_bias": 0.01 * nrm(ks[2], (L, N_BRANCHES * D_MODEL), jnp.float32),
        "w_attn_up": nrm(ks[3], (L, Q_WIDTH, D_MODEL), jnp.float32) * Q_WIDTH ** -0.5 * DEEPNORM_BETA,
        "w_conv_up": nrm(ks[4], (L, CONV_DIM, D_MODEL), jnp.float32) * CONV_DIM ** -0.5 * DEEPNORM_BETA,
        "conv_w": nrm(ks[5], (L, CONV_WIDTH, CONV_DIM), jnp.float32) * CONV_WIDTH ** -0.5,
        "w_out": nrm(ks[6], (L, D_MODEL, D_MODEL), jnp.float32) * D_MODEL ** -0.5 * DEEPNORM_BETA,
        "ln1_g": 1.0 + 0.02 * nrm(ks[7], (L, D_MODEL), jnp.float32),
        "ln1_b": 0.02 * nrm(ks[8], (L, D_MODEL), jnp.float32),
        "router_group_w": nrm(ks[9], (L, D_MODEL, N_GROUPS), jnp.float32) * D_MODEL ** -0.5,
        "router_group_b": 0.01 * nrm(ks[10], (L, N_GROUPS), jnp.float32),
        "router_expert_w": nrm(ks[11], (L, N_GROUPS, D_MODEL, EXPERTS_PER_GROUP), jnp.float32) * D_MODEL ** -0.5,
        "router_expert_b": 0.01 * nrm(ks[12], (L, N_GROUPS, EXPERTS_PER_GROUP), jnp.float32),
        "w_gate_e": nrm(ks[13], (L, N_EXPERTS, D_MODEL, D_EXPERT), jnp.float32) * D_MODEL ** -0.5,
        "w_up_e": nrm(ks[14], (L, N_EXPERTS, D_MODEL, D_EXPERT), jnp.float32) * D_MODEL ** -0.5,
        "w_down_e": nrm(ks[15], (L, N_EXPERTS, D_EXPERT, D_MODEL), jnp.float32) * D_EXPERT ** -0.5 * DEEPNORM_BETA,
        "ln2_g": 1.0 + 0.02 * nrm(ks[16], (L, D_MODEL), jnp.float32),
        "ln2_b": 0.02 * nrm(ks[17], (L, D_MODEL), jnp.float32),
    }


def reference(x, w_in, gate_bias, w_attn_up, w_conv_up, conv_w, w_out, ln1_g, ln1_b,
              router_group_w, router_group_b, router_expert_w, router_expert_b,
              w_gate_e, w_up_e, w_down_e, ln2_g, ln2_b):
    S = x.shape[1]
    top_k = min(TOPK_MAX, S // 4)
    pos = jnp.arange(S, dtype=jnp.int32)
    h = x
    for l in range(DEPTH):
        mix = token_mixer(h, pos, top_k, w_in[l], gate_bias[l], w_attn_up[l], w_conv_up[l],
                          conv_w[l], w_out[l])
        h = layer_norm(DEEPNORM_ALPHA * h + mix, ln1_g[l], ln1_b[l])
        ffn = hierarchical_moe(h, router_group_w[l], router_group_b[l], router_expert_w[l],
                               router_expert_b[l], w_gate_e[l], w_up_e[l], w_down_e[l])
        h = layer_norm(DEEPNORM_ALPHA * h + ffn, ln2_g[l], ln2_b[l])
    return h
```

```python
import math
import numpy as np
import ml_dtypes
import concourse.bass as bass
import concourse.mybir as mybir
from concourse.bass_utils import run_bass_kernel_spmd

F32 = mybir.dt.float32
BF16 = mybir.dt.bfloat16
I32 = mybir.dt.int32
ALU = mybir.AluOpType
AF = mybir.ActivationFunctionType

NCORES = 8
S = 2048
D = 1024
NT = 16
CAP = 384
NEXP = 32
ALPHA = 2.0 ** 0.25
LN_EPS = 1e-5
TOPK = 256
SEARCH_B = 128.0
SEARCH_IT = 19
NEG = -1.0e30

ENGS = ("pe", "act", "dve", "pool", "sp")


class T:
    __slots__ = ("t", "w", "r", "lanes", "name")

    def __init__(self, t, name=""):
        self.t = t
        self.w = {}
        self.r = {}
        self.lanes = {}
        self.name = name

    def __getitem__(self, k):
        return self.t[k]


class FW:
    def __init__(self, nc):
        self.nc = nc
        self.ops = {e: [] for e in ENGS}
        self.esem = {}
        self.ecnt = {e: 0 for e in ENGS}
        self.waited = {e: {} for e in ENGS}
        self.stack = []
        self.dma_ts = []
        for e in ENGS:
            self.esem[e] = nc.alloc_semaphore(name="c_" + e)

    def sb(self, name, shape, dtype):
        g = self.nc.sbuf_tensor("s_" + name, list(shape), dtype)
        tt = T(g.__enter__(), name)
        self.stack.append((tt, g))
        return tt

    def ps(self, name, shape, dtype):
        g = self.nc.psum_tensor("p_" + name, list(shape), dtype)
        tt = T(g.__enter__(), name)
        self.stack.append((tt, g))
        return tt

    def mark(self):
        return len(self.stack)

    def release(self, mark):
        while len(self.stack) > mark:
            tt, g = self.stack.pop()
            g.__exit__(None, None, None)

    def dram(self, name, shape, dtype, kind="Internal"):
        return T(self.nc.dram_tensor(name, list(shape), dtype, kind=kind), name)

    def _waits(self, e, reads, writes, nowaw=False):
        need = {}
        for t in reads:
            for s, v in t.w.items():
                need[s] = max(need.get(s, 0), v)
        for t in writes:
            if not nowaw:
                for s, v in t.w.items():
                    need[s] = max(need.get(s, 0), v)
            for s, v in t.r.items():
                need[s] = max(need.get(s, 0), v)
        out = []
        wd = self.waited[e]
        for s, v in need.items():
            if s is self.esem[e] and e in ("pe", "sp"):
                continue
            if wd.get(s, 0) >= v:
                continue
            wd[s] = v
            out.append((s, v))
        return out

    def op(self, e, fn, reads=(), writes=()):
        ws = self._waits(e, reads, writes)
        self.ecnt[e] += 1
        sem = self.esem[e]
        ev = (sem, self.ecnt[e])

        def run(eng, ws=ws, fn=fn, sem=sem):
            for s, v in ws:
                eng.wait_ge(s, v)
            fn(eng).then_inc(sem, 1)
        self.ops[e].append(run)
        for t in reads:
            t.r[sem] = max(t.r.get(sem, 0), ev[1])
        for t in writes:
            t.w = {sem: ev[1]}
            t.r = {}
        return ev

    def dma(self, q, out_t, in_t, fn, extra_reads=(), nowaw=False, lane=0):
        reads = [in_t] + list(extra_reads)
        ws = self._waits(q, reads, [out_t], nowaw=nowaw)
        if lane not in out_t.lanes:
            out_t.lanes[lane] = [self.nc.alloc_semaphore(name="d_%s_%s" % (out_t.name, lane)), 0]
            if out_t not in self.dma_ts:
                self.dma_ts.append(out_t)
        ln = out_t.lanes[lane]
        ln[1] += 16
        sem = ln[0]
        ev = (sem, ln[1])

        def run(eng, ws=ws, fn=fn, sem=sem):
            for s, v in ws:
                eng.wait_ge(s, v)
            fn(eng).then_inc(sem, 16)
        self.ops[q].append(run)
        for t in reads:
            t.r[sem] = max(t.r.get(sem, 0), ev[1])
        if nowaw:
            out_t.w[sem] = ev[1]
        else:
            out_t.w = {sem: ev[1]}
            out_t.r = {}
        return ev

    def barrier(self):
        evs = [(self.esem[e], self.ecnt[e]) for e in ENGS if self.ecnt[e] > 0]
        for t in self.dma_ts:
            evs += [(ln[0], ln[1]) for ln in t.lanes.values() if ln[1] > 0]
        for e in ENGS:
            ws = []
            wd = self.waited[e]
            for s, v in evs:
                if s is self.esem[e]:
                    continue
                if wd.get(s, 0) >= v:
                    continue
                wd[s] = v
                ws.append((s, v))

            def run(eng, ws=ws):
                for s, v in ws:
                    eng.wait_ge(s, v)
            self.ops[e].append(run)

    def wait_all(self, e, tiles):
        ws = self._waits(e, tiles, [])

        def run(eng, ws=ws):
            for s, v in ws:
                eng.wait_ge(s, v)
        self.ops[e].append(run)

    def finish(self):
        with self.nc.Block() as block:
            @block.tensor
            def _(eng):
                for f in self.ops["pe"]:
                    f(eng)

            @block.scalar
            def _(eng):
                for f in self.ops["act"]:
                    f(eng)

            @block.vector
            def _(eng):
                for f in self.ops["dve"]:
                    f(eng)

            @block.gpsimd
            def _(eng):
                for f in self.ops["pool"]:
                    f(eng)

            @block.sync
            def _(eng):
                for f in self.ops["sp"]:
                    f(eng)
        self.release(0)


def bview(t, n=128):
    return t[:].bitcast(BF16).rearrange("p (c t) -> p c t", t=n)


def build_program(stop_after=None, dbg=False):
    nc = bass.Bass("TRN2", target_bir_lowering=False)
    fw = FW(nc)
    EI = "ExternalInput"
    x_d = fw.dram("x", [2, S, D], F32, EI)
    wtm_d = fw.dram("w_tm", [D, 1352], F32, EI)
    wfm_d = fw.dram("w_fm", [D, 3584], F32, EI)
    cs_d = fw.dram("cs", [2, S, 8], F32, EI)
    const_d = fw.dram("consts", [128, 288], F32, EI)
    gb_d = fw.dram("gbias", [128, 16], F32, EI)
    convw_d = fw.dram("convw", [128, 4, 3], F32, EI)
    waup_d = fw.dram("w_aup", [512, D], F32, EI)
    wcup_d = fw.dram("w_cup", [512, D], F32, EI)
    wout_d = fw.dram("w_out", [D, D], F32, EI)
    lnp_d = fw.dram("lnp", [4, D], F32, EI)
    wr_d = fw.dram("w_r", [D, 36], F32, EI)
    rb_d = fw.dram("b_r", [1, 36], F32, EI)
    if stop_after is None:
        wg_d = fw.dram("w_gate", [NEXP, D, 512], F32, EI)
        wu_d = fw.dram("w_up", [NEXP, D, 512], F32, EI)
        wd_d = fw.dram("w_down", [NEXP, 512, D], F32, EI)
    zeros_d = fw.dram("zeros", [128, D], BF16, EI)
    out_d = fw.dram("out", [2, S, D], F32, "ExternalOutput")
    SK = "ExternalOutput" if dbg else "Internal"
    attnT_d = fw.dram("attnT_scr", [128, 4, 2 * S], BF16, SK)
    h_d = fw.dram("h_scr", [2 * S, D], F32, SK)
    xs_d = fw.dram("xs_scr", [NEXP * CAP, D], BF16, SK)
    y_d = fw.dram("y_scr", [NEXP * CAP, D], F32, SK)
    xT_d = fw.dram("xT_scr", [128, 8, 2 * S], BF16, "Internal")
    wgc_d = fw.dram("wg_bf", [NEXP, D, 512], BF16, "Internal")
    wuc_d = fw.dram("wu_bf", [NEXP, D, 512], BF16, "Internal")
    wdc_d = fw.dram("wd_bf", [NEXP, 512, D], BF16, "Internal")
    if dbg:
        dbg_feat = fw.dram("dbg_feat", [128, 14, S], BF16, "ExternalOutput")
        dbg_v = fw.dram("dbg_v", [128, NT, 2, 65], BF16, "ExternalOutput")
        dbg_wi = fw.dram("dbg_wi", [128, NT, 8], F32, "ExternalOutput")
        dbg_sc = fw.dram("dbg_sc", [NT, 128, S], F32, "ExternalOutput")
        dbg_mk = fw.dram("dbg_mk", [NT, 128, S], BF16, "ExternalOutput")
        dbg_rt = fw.dram("dbg_rt", [128, 32, 8], F32, "ExternalOutput")

    P01 = fw.ps("P01", [128, 1024], F32)
    P23 = fw.ps("P23", [128, 1024], F32)
    P4 = fw.ps("P4", [128, 512], F32)
    P5 = fw.ps("P5", [128, 512], F32)
    P6 = fw.ps("P6", [128, 512], F32)
    P7 = fw.ps("P7", [128, 512], F32)
    Q0, Q1, Q2, Q3 = T(P01.t, "Q0"), T(P01.t, "Q1"), T(P23.t, "Q2"), T(P23.t, "Q3")
    QAP = {Q0: P01[:, 0:512], Q1: P01[:, 512:1024], Q2: P23[:, 0:512], Q3: P23[:, 512:1024]}

    consts = fw.sb("consts", [128, 288], F32)
    fw.dma("sp", consts, const_d, lambda e: e.dma_start(out=consts[:], in_=const_d.t.ap()))
    ident_b = fw.sb("ident_b", [128, 128], BF16)
    fw.op("dve", lambda e: e.tensor_copy(out=ident_b[:], in_=consts[:, 0:128]), reads=[consts], writes=[ident_b])
    ones_f = fw.sb("ones_f", [128, 128], F32)
    fw.op("dve", lambda e: e.memset(ones_f[:], 1.0), writes=[ones_f])
    ident_f = consts

    def IDF():
        return consts[:, 0:128]

    def TRI():
        return consts[:, 128:256]

    def ECAP():
        return consts[:, 256:288]

    dest_all = fw.sb("dest_all", [128, 32, 2], I32)
    gate_all = fw.sb("gate_all", [128, 32, 2], F32)
    base_mark = fw.mark()

    wtm = fw.sb("wtm", [128, 8, 1352], BF16)
    fw.dma("pool", wtm, wtm_d, lambda e: e.dma_start(out=wtm[:], in_=wtm_d.t.ap().rearrange("(c p) n -> p c n", p=128)))
    cs_sb = fw.sb("cs_sb", [128, 2, NT, 8], F32)
    for a in range(2):
        fw.dma("sp", cs_sb, cs_d, lambda e, a=a: e.dma_start(
            out=cs_sb[:, a, :, :], in_=cs_d.t[a].rearrange("(t p) i -> p t i", p=128)))
    xst = [fw.sb(f"xst{i}", [128, D], F32) for i in range(2)]
    xTt = [fw.sb(f"xTt{i}", [128, 8, 128], BF16) for i in range(2)]
    qq = [fw.sb(f"qq{i}", [128, 16, 64], BF16) for i in range(2)]
    kk1 = fw.sb("kkone", [128, 3, 64], BF16)
    kk = [fw.sb(f"kk{i}", [128, 6, 2, 64], BF16) for i in range(2)]
    for _k in kk:
        fw.op("pool", lambda e, _k=_k: e.memset(_k[:], 0.0), writes=[_k])
    rt = [fw.sb(f"rt{i}", [128, 16, 8], F32) for i in range(4)]
    rk = [fw.sb(f"rk{i}", [128, 3, 8], F32) for i in range(4)]
    featT = fw.sb("featT", [128, 14, S], BF16)
    V_aug = fw.sb("V_aug", [128, NT, 2, 65], BF16)
    wi_sb = fw.sb("wi_sb", [128, NT, 8], F32)
    sc = [fw.sb(f"sc{i}", [128, S], F32) for i in range(4)]
    rbuf = [fw.sb(f"rbuf{i}", [128, 2, 512], BF16) for i in range(3)]
    dg = [fw.sb(f"dg{i}", [128, 8, 128], BF16) for i in range(2)]
    mk = [fw.sb(f"mk{i}", [128, S], BF16) for i in range(2)]
    junkD, junkA = mk[0], mk[1]
    cnt4 = fw.sb("cnt4", [128, 4], F32)
    cntT = [T(cnt4.t, f"cnt4_{i}") for i in range(4)]
    fw.op("dve", lambda e: e.memset(cnt4[:], 0.0), writes=cntT)
    g4 = fw.sb("g4", [128, 4], F32)
    d8 = fw.sb("d8", [128, 2, 4], F32)
    st8 = fw.sb("st8", [128, 2, 4], F32)
    thr4 = fw.sb("thr4", [128, 4], F32)
    stT = [T(st8.t, f"st8_{i}") for i in range(2)]
    gT = [T(g4.t, f"g4_{i}") for i in range(2)]
    dT = [T(d8.t, f"d8_{i}") for i in range(2)]
    lo4 = fw.sb("lo4", [128, 4], F32)
    ssT = fw.sb("ssT", [128, SEARCH_IT, 2, 4], F32)
    _stp = SEARCH_B
    for _it in range(SEARCH_IT):
        fw.op("pool", lambda e, _it=_it, _stp=_stp: e.memset(ssT[:, _it, 0, :], float(_stp)), writes=[ssT])
        fw.op("pool", lambda e, _it=_it, _stp=_stp: e.memset(ssT[:, _it, 1, :], float(-_stp)), writes=[ssT])
        _stp = _stp * 0.5
    negb = fw.sb("negb", [128, 1], F32)
    fw.op("pool", lambda e: e.memset(negb[:], -30000.0), writes=[negb])
    mT = [fw.sb(f"mT{i}", [128, NT, 512], BF16) for i in range(2)]
    ebuf = [fw.sb(f"ebuf{i}", [128, 2, 512], BF16) for i in range(3)]
    rec = fw.sb("rec", [128, 4], F32)
    attn_tm = [fw.sb(f"attn_tm{i}", [128, 4, 512], BF16) for i in range(1)]
    attnT_c = [fw.sb(f"attnT_c{i}", [128, 4, 512], BF16) for i in range(2)]

    fw.op("pool", lambda e: e.memset(V_aug[:], 1.0), writes=[V_aug])

    ctr = {"pab": 0, "acc": 0, "r": 0, "m": 0, "e": 0}

    def nxt(key, n):
        v = ctr[key] % n
        ctr[key] += 1
        return v

    def rope(src3, nh, tt, tmps, dst3, srcT):
        cosb = cs_sb[:, 0, tt, :].unsqueeze(1).to_broadcast([128, nh, 8])
        sinb = cs_sb[:, 1, tt, :].unsqueeze(1).to_broadcast([128, nh, 8])
        t1, t2, t3, t4 = tmps
        x1 = src3[:, :, 0:8]
        x2 = src3[:, :, 8:16]
        fw.op("dve", lambda e: e.tensor_tensor(out=t1[:], in0=x1, in1=cosb, op=ALU.mult), reads=[srcT, cs_sb], writes=[t1])
        fw.op("dve", lambda e: e.tensor_tensor(out=t2[:], in0=x2, in1=sinb, op=ALU.mult), reads=[srcT, cs_sb], writes=[t2])
        fw.op("dve", lambda e: e.tensor_tensor(out=t3[:], in0=x2, in1=cosb, op=ALU.mult), reads=[srcT, cs_sb], writes=[t3])
        fw.op("dve", lambda e: e.tensor_tensor(out=t4[:], in0=x1, in1=sinb, op=ALU.mult), reads=[srcT, cs_sb], writes=[t4])
        return (t1, t2, t3, t4)

    for b in range(2):
        def a1_load(tt, b=b):
            xs_ = xst[tt % 2]
            fw.dma("sp", xs_, x_d, lambda e, xs_=xs_, tt=tt, b=b: e.dma_start(out=xs_[:], in_=x_d.t[b, tt * 128:(tt + 1) * 128, :]))

        def a1_tr(tt, b=b):
            xs_ = xst[tt % 2]
            xT = xTt[tt % 2]

            def tr(e, xs_=xs_):
                for dc in range(8):
                    Pq = P5 if dc < 4 else P6
                    i = e.transpose(Pq[:, (dc % 4) * 128:(dc % 4 + 1) * 128], xs_[:, dc * 128:(dc + 1) * 128], IDF())
                return i
            fw.op("pe", tr, reads=[xs_, consts], writes=[P5, P6])
            fw.op("act", lambda e, xT=xT: e.activation(out=xT[:, 0:4, :], in_=P5[:].rearrange("p (c t) -> p c t", t=128), func=AF.Copy),
                  reads=[P5], writes=[xT])
            fw.op("dve", lambda e, xT=xT: e.tensor_copy(out=xT[:, 4:8, :], in_=P6[:].rearrange("p (c t) -> p c t", t=128)),
                  reads=[P6], writes=[xT])
            tg = b * NT + tt
            fw.dma("sp", xT_d, xT, lambda e, xT=xT, tg=tg: e.dma_start(out=xT_d.t[:, :, tg * 128:(tg + 1) * 128], in_=xT[:]),
                   nowaw=True, lane=tt % 2)

        def a1_proj_rope(tt):
            xT = xTt[tt % 2]
            q_ = qq[tt % 2]
            k_ = kk[tt % 2]
            PQ, PK = ((P23, P4), (P01, P7))[tt % 2]

            def proj(e, xT=xT, PQ=PQ, PK=PK):
                for (dst, c0, n) in ((PQ[:, 0:512], 0, 512), (PQ[:, 512:1024], 512, 512), (PK[:, 0:328], 1024, 328)):
                    for dc in range(8):
                        i = e.matmul(dst, lhsT=xT[:, dc, :], rhs=wtm[:, dc, c0:c0 + n], start=(dc == 0), stop=(dc == 7))
                return i
            fw.op("pe", proj, reads=[xT, wtm], writes=[PQ, PK])

        def a1_rope(tt):
            q_ = qq[tt % 2]
            k_ = kk[tt % 2]
            PQ, PK = ((P23, P4), (P01, P7))[tt % 2]
            v16 = PQ[:].rearrange("p (h d) -> p h d", d=64)
            v3 = PK[:, 0:192].rearrange("p (h d) -> p h d", d=64)
            t1, t2, t3, t4 = rope(v16, 16, tt, rt, None, PQ)
            fw.op("dve", lambda e, q_=q_: e.tensor_tensor(out=q_[:, :, 0:8], in0=t1[:], in1=t2[:], op=ALU.subtract),
                  reads=[t1, t2], writes=[q_])
            fw.op("dve", lambda e, q_=q_: e.tensor_tensor(out=q_[:, :, 8:16], in0=t3[:], in1=t4[:], op=ALU.add),
                  reads=[t3, t4], writes=[q_])
            fw.op("act", lambda e, q_=q_, v16=v16: e.activation(out=q_[:, :, 16:64], in_=v16[:, :, 16:64], func=AF.Copy),
                  reads=[PQ], writes=[q_])
            s1, s2, s3, s4 = rope(v3, 3, tt, rk, None, PK)
            fw.op("dve", lambda e: e.tensor_tensor(out=kk1[:, :, 0:8], in0=s1[:], in1=s2[:], op=ALU.subtract),
                  reads=[s1, s2], writes=[kk1])
            fw.op("dve", lambda e: e.tensor_tensor(out=kk1[:, :, 8:16], in0=s3[:], in1=s4[:], op=ALU.add),
                  reads=[s3, s4], writes=[kk1])
            fw.op("act", lambda e, v3=v3: e.activation(out=kk1[:, :, 16:64], in_=v3[:, :, 16:64], func=AF.Copy),
                  reads=[PK], writes=[kk1])
            kv5 = k_[:].rearrange("p (h a) r d -> p h a r d", a=2)
            fw.op("pool", lambda e, kv5=kv5: e.tensor_copy(out=kv5[:, :, 0, 0, :], in_=kk1[:, :, :]), reads=[kk1], writes=[k_])
            fw.op("pool", lambda e, kv5=kv5: e.tensor_copy(out=kv5[:, :, 1, 1, :], in_=kk1[:, :, :]), reads=[kk1], writes=[k_])
            fw.op("act", lambda e, tt=tt, PK=PK: e.activation(out=V_aug[:, tt, :, 0:64],
                                                             in_=PK[:, 192:320].rearrange("p (h d) -> p h d", d=64), func=AF.Copy),
                  reads=[PK], writes=[V_aug])
            fw.op("act", lambda e, tt=tt, PK=PK: e.activation(out=wi_sb[:, tt, :], in_=PK[:, 320:328], func=AF.Copy),
                  reads=[PK], writes=[wi_sb])

        def a1_tr2(tt):
            q_ = qq[tt % 2]
            k_ = kk[tt % 2]

            def tr2(e, q_=q_, k_=k_):
                q2 = q_[:].rearrange("p h d -> p (h d)")
                k2 = k_[:].rearrange("p h r d -> p (h r d)")
                for c in range(8):
                    i = e.transpose(bview(P5)[:, c, :], q2[:, c * 128:(c + 1) * 128], ident_b[:])
                for c in range(6):
                    i = e.transpose(bview(P6)[:, c, :], k2[:, c * 128:(c + 1) * 128], ident_b[:])
                return i
            fw.op("pe", tr2, reads=[q_, k_, ident_b], writes=[P5, P6])
            fw.op("act", lambda e, tt=tt: e.activation(out=featT[:, 0:8, tt * 128:(tt + 1) * 128], in_=bview(P5), func=AF.Copy),
                  reads=[P5], writes=[featT])
            fw.op("dve", lambda e, tt=tt: e.tensor_copy(out=featT[:, 8:14, tt * 128:(tt + 1) * 128], in_=bview(P6)[:, 0:6, :]),
                  reads=[P6], writes=[featT])

        a1_load(0)
        a1_load(1)
        a1_tr(0)
        for tt in range(NT):
            a1_proj_rope(tt)
            if tt + 1 < NT:
                a1_tr(tt + 1)
            a1_rope(tt)
            if tt + 2 < NT:
                a1_load(tt + 2)
            if tt >= 1:
                a1_tr2(tt - 1)
        a1_tr2(NT - 1)
        if dbg and b == 0:
            fw.dma("sp", dbg_feat, featT, lambda e: e.dma_start(out=dbg_feat.t.ap(), in_=featT[:]))
            fw.dma("sp", dbg_v, V_aug, lambda e: e.dma_start(out=dbg_v.t.ap(), in_=V_aug[:]))
            fw.dma("sp", dbg_wi, wi_sb, lambda e: e.dma_start(out=dbg_wi.t.ap(), in_=wi_sb[:]))
        if stop_after == "A1":
            break

        def gen_indexer(c, b=b):
            steps = []
            for j in range(4 * c, 4 * c + 4):
                L = 128 * (j + 1)
                for n in range((L + 511) // 512):
                    for cp in range(4):
                        steps.append((j, n, cp, min(512, L - n * 512)))
            pend = None
            acc_of = {}

            def emit_mmd(st):
                j, n, cp, N, r_, dg_ = st
                acc = acc_of[(j, n)]

                def mmd(e, acc=acc, dg_=dg_, r_=r_, cp=cp, N=N):
                    e.matmul(acc[:, 0:N], lhsT=dg_[:, 2 * cp, :], rhs=r_[:, 0, 0:N], start=(cp == 0), stop=False)
                    return e.matmul(acc[:, 0:N], lhsT=dg_[:, 2 * cp + 1, :], rhs=r_[:, 1, 0:N], start=False, stop=(cp == 3))
                fw.op("pe", mmd, reads=[dg_, r_], writes=[acc])
                if cp == 3:
                    sc_ = sc[j % 4]
                    fw.op("dve", lambda e, acc=acc, sc_=sc_, n=n, N=N: e.tensor_copy(out=sc_[:, n * 512:n * 512 + N], in_=acc[:, 0:N]),
                          reads=[acc], writes=[sc_])
                    if (n + 1) * 512 >= 128 * (j + 1):
                        fw.op("pool", lambda e, sc_=sc_, j=j: e.affine_select(
                            out=sc_[:, j * 128:(j + 1) * 128], in_=sc_[:, j * 128:(j + 1) * 128], pattern=[[-1, 128]],
                            compare_op=ALU.is_ge, fill=NEG, base=0, channel_multiplier=1), reads=[sc_], writes=[sc_])
                        if dbg and b == 0:
                            fw.dma("sp", dbg_sc, sc_, lambda e, sc_=sc_, j=j: e.dma_start(out=dbg_sc.t[j], in_=sc_[:]), nowaw=True, lane=j % 4)
            last_j = None
            for k, (j, n, cp, N) in enumerate(steps):
                dg_ = dg[j % 2]
                if j != last_j:
                    for h in range(8):
                        fw.op("pool", lambda e, h=h, dg_=dg_, j=j: e.tensor_scalar(
                            out=dg_[:, h, :], in0=ident_b[:], scalar1=wi_sb[:, j, h:h + 1], scalar2=0.0,
                            op0=ALU.mult, op1=ALU.add), reads=[ident_b, wi_sb], writes=[dg_])
                    last_j = j
                if cp == 0:
                    acc_of[(j, n)] = (P4, P7)[nxt("acc", 2)]
                Pab = (P01, P23)[nxt("pab", 2)]
                r_ = rbuf[nxt("r", 3)]

                def mmi(e, Pab=Pab, cp=cp, j=j, n=n, N=N):
                    e.matmul(Pab[:, 0:N], lhsT=featT[:, 4 + cp, j * 128:(j + 1) * 128],
                             rhs=featT[:, 12, n * 512:n * 512 + N], start=True, stop=True)
                    return e.matmul(Pab[:, 512:512 + N], lhsT=featT[:, 4 + cp, j * 128:(j + 1) * 128],
                                    rhs=featT[:, 13, n * 512:n * 512 + N], start=True, stop=True)
                fw.op("pe", mmi, reads=[featT], writes=[Pab])
                if k % 2 == 0:
                    fw.op("act", lambda e, Pab=Pab, r_=r_, N=N: e.activation(
                        out=r_[:, :, 0:N], in_=Pab[:].rearrange("p (a n) -> p a n", a=2)[:, :, 0:N], func=AF.Relu),
                        reads=[Pab], writes=[r_])
                else:
                    fw.op("dve", lambda e, Pab=Pab, r_=r_, N=N: e.tensor_scalar(
                        out=r_[:, :, 0:N], in0=Pab[:].rearrange("p (a n) -> p a n", a=2)[:, :, 0:N], scalar1=0.0, scalar2=None,
                        op0=ALU.max), reads=[Pab], writes=[r_])
                if pend is not None:
                    emit_mmd(pend)
                pend = (j, n, cp, N, r_, dg_)
                yield
            emit_mmd(pend)
            yield

        def gen_search(c, b=b):
            mTc = mT[c % 2]
            js = list(range(4 * c, 4 * c + 4))
            Q = [jq for jq in range(4) if js[jq] >= 2]
            dveq = Q[:len(Q) // 2]
            actq = Q[len(Q) // 2:]
            for jq in range(4):
                L = 128 * (js[jq] + 1)
                val = (TOPK - 0.5) if jq in dveq else (2.0 * TOPK - 1.0 - L)
                fw.op("pool", lambda e, jq=jq, val=val: e.memset(thr4[:, jq:jq + 1], float(val)), writes=[thr4])
            fw.op("dve", lambda e: e.memset(st8[:], 0.0), writes=stT)
            for jq in range(4):
                if jq not in Q:
                    fw.op("dve", lambda e, jq=jq: e.memset(lo4[:, jq:jq + 1], -1.0e29), writes=[lo4])
            yield

            def colv(ap4, ch):
                return ap4.rearrange("p (a b) -> p a b", b=2)[:, :, ch]

            def colv3(ap8, ch):
                return ap8.rearrange("p s (a b) -> p s a b", b=2)[:, :, :, ch]
            step = SEARCH_B
            for it in range(SEARCH_IT):
                for jq in Q:
                    L = 128 * (js[jq] + 1)
                    sc_ = sc[js[jq] % 4]
                    ch = jq % 2
                    if jq in dveq:
                        fw.op("dve", lambda e, sc_=sc_, L=L, jq=jq: e.tensor_scalar(
                            out=junkD[:, 0:L], in0=sc_[:, 0:L], scalar1=st8[:, 0, jq:jq + 1], scalar2=None,
                            op0=ALU.is_ge, op1=ALU.add, accum_out=cnt4[:, jq:jq + 1]), reads=[sc_, stT[ch]], writes=[cntT[jq], junkD])
                    else:
                        fw.op("act", lambda e, sc_=sc_, L=L, jq=jq: e.activation(
                            out=junkA[:, 0:L], in_=sc_[:, 0:L], func=AF.Sign, bias=st8[:, 1, jq:jq + 1], scale=1.0,
                            accum_out=cnt4[:, jq:jq + 1]), reads=[sc_, stT[ch]], writes=[cntT[jq], junkA])
                for ch in range(2):
                    if not any(jq % 2 == ch for jq in Q):
                        continue
                    fw.op("dve", lambda e, ch=ch: e.tensor_tensor(out=colv(g4[:], ch), in0=colv(cnt4[:], ch), in1=colv(thr4[:], ch), op=ALU.is_ge),
                          reads=[cntT[ch], cntT[ch + 2], thr4], writes=[gT[ch]])
                    if it < SEARCH_IT - 1:
                        fw.op("dve", lambda e, it=it, ch=ch: e.scalar_tensor_tensor(
                            out=colv3(d8[:], ch), in0=colv(g4[:], ch).unsqueeze(1).to_broadcast([128, 2, 2]), scalar=-0.5,
                            in1=colv3(ssT[:, it, :, :], ch), op0=ALU.add, op1=ALU.mult), reads=[gT[ch], ssT], writes=[dT[ch]])
                        fw.op("dve", lambda e, ch=ch: e.tensor_tensor(out=colv3(st8[:], ch), in0=colv3(st8[:], ch), in1=colv3(d8[:], ch), op=ALU.add),
                              reads=[stT[ch], dT[ch]], writes=[stT[ch]])
                    else:
                        fw.op("dve", lambda e, step=step, ch=ch: e.tensor_scalar(out=colv(g4[:], ch), in0=colv(g4[:], ch), scalar1=-1.0, scalar2=step,
                                                                                 op0=ALU.add, op1=ALU.mult), reads=[gT[ch]], writes=[gT[ch]])
                        for jq in Q:
                            if jq % 2 == ch:
                                fw.op("dve", lambda e, jq=jq: e.tensor_tensor(out=lo4[:, jq:jq + 1], in0=g4[:, jq:jq + 1], in1=st8[:, 0, jq:jq + 1], op=ALU.add),
                                      reads=[gT[ch], stT[ch]], writes=[lo4])
                step = step * 0.5
                yield
            for jq in range(4):
                j = js[jq]
                L = 128 * (j + 1)
                sc_ = sc[j % 4]
                mk_ = mk[j % 2]
                fw.op("dve", lambda e, sc_=sc_, mk_=mk_, L=L, jq=jq: e.tensor_scalar(
                    out=mk_[:, 0:L], in0=sc_[:, 0:L], scalar1=lo4[:, jq:jq + 1], scalar2=None, op0=ALU.is_ge),
                    reads=[sc_, lo4], writes=[mk_])
                if dbg and b == 0:
                    fw.dma("sp", dbg_mk, mk_, lambda e, mk_=mk_, j=j: e.dma_start(out=dbg_mk.t[j], in_=mk_[:]), nowaw=True, lane=j % 2)
                for g0 in range(0, j + 1, 8):
                    ng = min(8, j + 1 - g0)
                    Pm = (P5, P6)[nxt("m", 2)]

                    def trm(e, Pm=Pm, mk_=mk_, g0=g0, ng=ng):
                        for i in range(ng):
                            ins = e.transpose(bview(Pm)[:, i, :], mk_[:, (g0 + i) * 128:(g0 + i + 1) * 128], ident_b[:])
                        return ins
                    fw.op("pe", trm, reads=[mk_, ident_b], writes=[Pm])
                    fw.op("dve", lambda e, Pm=Pm, mTc=mTc, g0=g0, ng=ng, j=j: e.tensor_scalar(
                        out=mTc[:, g0:g0 + ng, (j % 4) * 128:(j % 4 + 1) * 128], in0=bview(Pm)[:, 0:ng, :],
                        scalar1=30000.0, scalar2=-30000.0, op0=ALU.mult, op1=ALU.add), reads=[Pm], writes=[mTc])
                yield

        def gen_attn(c, b=b, solo=False):
            mTc = mT[c % 2]
            at_ = attn_tm[0]
            n_s = 4 * c + 4
            for hp in range(4):
                kvh = hp // 2
                pend = None

                def emit_pv(st, kvh=kvh):
                    i, p_ = st

                    def mmpv(e, p_=p_, i=i, kvh=kvh, c=c):
                        ins = None
                        for hh, O in ((0, P4), (1, P7)):
                            for jj in range(4):
                                if i <= 4 * c + jj:
                                    ins = e.matmul(O[:, jj * 65:(jj + 1) * 65], lhsT=p_[:, hh, jj * 128:(jj + 1) * 128],
                                                   rhs=V_aug[:, i, kvh, :], start=(i == 0 and jj == 0),
                                                   stop=(i == 4 * c + jj), skip_group_check=True)
                        return ins
                    fw.op("pe", mmpv, reads=[p_, V_aug], writes=[P4, P7])
                for i in range(n_s):
                    off = max(0, i - 4 * c) * 128
                    Pab = (P01, P23)[nxt("pab", 2)]
                    e_ = ebuf[nxt("e", 3)]

                    def mms(e, Pab=Pab, i=i, off=off, hp=hp, kvh=kvh, c=c, mTc=mTc, solo=solo):
                        e.matmul(Pab[:, off:512], lhsT=featT[:, 8 + 2 * kvh, i * 128:(i + 1) * 128],
                                 rhs=featT[:, hp, c * 512 + off:(c + 1) * 512], start=True, stop=solo)
                        ins = e.matmul(Pab[:, 512 + off:1024], lhsT=featT[:, 9 + 2 * kvh, i * 128:(i + 1) * 128],
                                       rhs=featT[:, hp, c * 512 + off:(c + 1) * 512], start=True, stop=solo)
                        if solo:
                            return ins
                        e.matmul(Pab[:, off:512], lhsT=ident_b[:], rhs=mTc[:, i, off:512], start=False, stop=True)
                        return e.matmul(Pab[:, 512 + off:1024], lhsT=ident_b[:], rhs=mTc[:, i, off:512], start=False, stop=True)
                    fw.op("pe", mms, reads=[featT, mTc, ident_b], writes=[Pab])
                    if solo:
                        N_ = 512 - off
                        fw.op("dve", lambda e, Pab=Pab, i=i, off=off, N_=N_, mTc=mTc: e.tensor_tensor(
                            out=Pab[:].rearrange("p (a n) -> p a n", a=2)[:, :, off:512],
                            in0=Pab[:].rearrange("p (a n) -> p a n", a=2)[:, :, off:512],
                            in1=mTc[:, i, off:512].unsqueeze(1).to_broadcast([128, 2, N_]), op=ALU.add),
                            reads=[Pab, mTc], writes=[Pab])
                    fw.op("act", lambda e, Pab=Pab, e_=e_, off=off: e.activation(
                        out=e_[:, :, off:512], in_=Pab[:].rearrange("p (a n) -> p a n", a=2)[:, :, off:512],
                        func=AF.Exp, scale=0.125), reads=[Pab], writes=[e_])
                    if pend is not None:
                        emit_pv(pend)
                    pend = (i, e_)
                    yield
                emit_pv(pend)
                for hh, O in ((0, P4), (1, P7)):
                    hd = 2 * hp + hh
                    O3 = O[:, 0:260].rearrange("p (j d) -> p j d", d=65)
                    fw.op("dve", lambda e, O3=O3: e.reciprocal(out=rec[:], in_=O3[:, :, 64]), reads=[O], writes=[rec])
                    fw.op("dve", lambda e, O3=O3, hd=hd, at_=at_: e.tensor_tensor(
                        out=at_[:, :, hd * 64:(hd + 1) * 64], in0=O3[:, :, 0:64],
                        in1=rec[:].unsqueeze(2).to_broadcast([128, 4, 64]), op=ALU.mult), reads=[O, rec], writes=[at_])
                yield
            aT_ = attnT_c[c % 2]
            for jj in range(4):
                Pm = (P5, P6)[nxt("m", 2)]

                def tra(e, Pm=Pm, jj=jj, at_=at_):
                    for hc in range(4):
                        ins = e.transpose(bview(Pm)[:, hc, :], at_[:, jj, hc * 128:(hc + 1) * 128], ident_b[:])
                    return ins
                fw.op("pe", tra, reads=[at_, ident_b], writes=[Pm])
                fw.op("act", lambda e, Pm=Pm, jj=jj, aT_=aT_: e.activation(
                    out=aT_[:, :, jj * 128:(jj + 1) * 128], in_=bview(Pm)[:, 0:4, :], func=AF.Copy), reads=[Pm], writes=[aT_])
            g = b * 4 + c
            fw.dma("sp", attnT_d, aT_, lambda e, g=g, aT_=aT_: e.dma_start(out=attnT_d.t[:, :, g * 512:(g + 1) * 512], in_=aT_[:]),
                   nowaw=True, lane=c % 2)
            yield

        def run(gen):
            for _ in gen:
                pass

        def interleave(ga, na, gb, nb):
            da = db = False
            ia = ib = 0
            while not (da and db):
                ta = (ia + 1) * nb
                tb = (ib + 1) * na
                if not da and (db or ta <= tb):
                    try:
                        next(ga)
                        ia += 1
                    except StopIteration:
                        da = True
                else:
                    try:
                        next(gb)
                        ib += 1
                    except StopIteration:
                        db = True

        def precast(ex):
            for src, dst in ((wg_d, wgc_d), (wu_d, wuc_d), (wd_d, wdc_d)):
                fw.dma("pool", dst, src, lambda e, src=src, dst=dst, ex=ex: e.dma_start(out=dst.t[ex], in_=src.t[ex]),
                       nowaw=True, lane="c")
        PC = (2, 4, 5, 5)
        pc0 = 16 * b

        def precast_chunk(c, b=b):
            z0 = (b * 4 + c) * 12
            for zi in range(z0, z0 + 12):
                fw.dma("pool", xs_d, zeros_d, lambda e, zi=zi: e.dma_start(out=xs_d.t[zi * 128:(zi + 1) * 128, :], in_=zeros_d.t.ap()),
                       nowaw=True, lane="z")
            if stop_after is None:
                for ex in range(pc0 + sum(PC[:c]), pc0 + sum(PC[:c + 1])):
                    precast(ex)

        precast_chunk(0)
        run(gen_indexer(0))
        run(gen_search(0))
        for c in range(1, 4):
            precast_chunk(c)
            run(gen_indexer(c))
            if stop_after == "A2":
                run(gen_search(c))
            else:
                interleave(gen_search(c), SEARCH_IT + 6, gen_attn(c - 1), 16 * c + 6)
        if stop_after != "A2":
            run(gen_attn(3, solo=False))

    outs = [out_d]
    if stop_after in ("A1", "A2", "A3"):
        tail = [attnT_d]
        if dbg:
            tail += [dbg_feat, dbg_v, dbg_wi, dbg_sc, dbg_mk]
        fw.wait_all("sp", [t for t in tail if t.w])
        fw.finish()
        return nc

    fw.barrier()
    fw.release(base_mark)

    wfm = fw.sb("wfm", [128, 8, 3584], BF16)
    for half in range(2):
        for ch in range(2):
            fw.dma("pool", wfm, wfm_d, lambda e, half=half, ch=ch: e.dma_start(
                out=wfm[:, half * 4:(half + 1) * 4, ch * 1792:(ch + 1) * 1792],
                in_=wfm_d.t[half * 512:(half + 1) * 512, ch * 1792:(ch + 1) * 1792].rearrange("(c p) n -> p c n", p=128)),
                nowaw=(half + ch > 0), lane=half * 2 + ch)
    waup = fw.sb("waup", [128, 4, D], BF16)
    wcup = fw.sb("wcup", [128, 4, D], BF16)
    wout = fw.sb("wout", [128, 8, D], BF16)
    fw.dma("pool", waup, waup_d, lambda e: e.dma_start(out=waup[:], in_=waup_d.t.ap().rearrange("(c p) n -> p c n", p=128)))
    fw.dma("pool", wcup, wcup_d, lambda e: e.dma_start(out=wcup[:], in_=wcup_d.t.ap().rearrange("(c p) n -> p c n", p=128)))
    fw.dma("pool", wout, wout_d, lambda e: e.dma_start(out=wout[:], in_=wout_d.t.ap().rearrange("(c p) n -> p c n", p=128)))
    gb = fw.sb("gb", [128, 16], F32)
    fw.dma("sp", gb, gb_d, lambda e: e.dma_start(out=gb[:], in_=gb_d.t.ap()))
    cw = fw.sb("cw", [128, 4, 3], F32)
    fw.dma("sp", cw, convw_d, lambda e: e.dma_start(out=cw[:], in_=convw_d.t.ap()))
    ln1g = fw.sb("ln1g", [128, D], F32)
    ln1b = fw.sb("ln1b", [128, D], F32)
    fw.dma("sp", ln1g, lnp_d, lambda e: e.dma_start(out=ln1g[:], in_=lnp_d.t[0:1, :].partition_broadcast(128)))
    fw.dma("sp", ln1b, lnp_d, lambda e: e.dma_start(out=ln1b[:], in_=lnp_d.t[1:2, :].partition_broadcast(128)))
    wr = fw.sb("wr", [128, 8, 36], F32)
    fw.dma("sp", wr, wr_d, lambda e: e.dma_start(out=wr[:], in_=wr_d.t.ap().rearrange("(c p) n -> p c n", p=128)))
    rb = fw.sb("rb", [128, 36], F32)
    fw.dma("sp", rb, rb_d, lambda e: e.dma_start(out=rb[:], in_=rb_d.t[0:1, :].partition_broadcast(128)))
    xs4 = fw.sb("xs4", [128, 4, D], F32)
    xs4t = [T(xs4.t, f"xs4_{i}") for i in range(4)]
    xTc = fw.sb("xTc", [128, 8, 512], BF16)
    aTc = [fw.sb(f"aTc{i}", [128, 4, 512], BF16) for i in range(1)]
    cin2 = [fw.sb(f"cin{i}", [128, 512], F32) for i in range(2)]
    u1 = fw.sb("u1", [128, 4, 514], F32)
    ycv2 = [fw.sb(f"ycv{i}", [128, 512], F32) for i in range(2)]
    convT = fw.sb("convT", [128, 4, 512], BF16)
    gA2 = [fw.sb(f"gA{i}", [128, 512], F32) for i in range(2)]
    gC2 = [fw.sb(f"gC{i}", [128, 512], F32) for i in range(2)]
    m12 = [fw.sb(f"m1{i}", [128, 512], F32) for i in range(2)]
    m22 = [fw.sb(f"m2{i}", [128, 512], F32) for i in range(2)]
    mergedT = fw.sb("mergedT", [128, 8, 512], BF16)
    ys = [fw.sb(f"ys{i}", [128, D], F32) for i in range(4)]
    hbf = [fw.sb(f"hbf{i}", [128, D], BF16) for i in range(4)]
    hT2 = [fw.sb(f"hT{i}", [128, 8, 128], F32) for i in range(1)]
    st64 = fw.sb("st64", [128, 4, 2, 6], F32)
    mv4 = fw.sb("mv4", [128, 4, 2], F32)
    sd4 = fw.sb("sd4", [128, 4], F32)
    rstd4 = fw.sb("rstd4", [128, 4], F32)
    nmr4 = fw.sb("nmr4", [128, 4], F32)
    epsb = fw.sb("epsb", [128, 1], F32)
    fw.op("dve", lambda e: e.memset(epsb[:], LN_EPS), writes=[epsb])
    lg4 = fw.sb("lg4", [128, 4, 36], F32)
    gmx4 = fw.sb("gmx4", [128, 4], F32)
    ohg4 = fw.sb("ohg4", [128, 4, 4], F32)
    sh4 = fw.sb("sh4", [128, 4, 4], F32)
    seg4 = fw.sb("seg4", [128, 4], F32)
    pgrp4 = fw.sb("pgrp4", [128, 4], F32)
    prod4 = fw.sb("prod4", [128, 4, 4, 8], F32)
    esel4 = fw.sb("esel4", [128, 4, 8], F32)
    mx14 = fw.sb("mx14", [128, 4], F32)
    mx24 = fw.sb("mx24", [128, 4], F32)
    oh14 = fw.sb("oh14", [128, 4, 8], F32)
    oh24 = fw.sb("oh24", [128, 4, 8], F32)
    e24 = fw.sb("e24", [128, 4, 8], F32)
    ed4 = fw.sb("ed4", [128, 4], F32)
    w14 = fw.sb("w14", [128, 4], F32)
    E14 = fw.sb("E14", [128, 4, 4, 8], F32)
    E24 = fw.sb("E24", [128, 4, 4, 8], F32)
    Mm4 = fw.sb("Mm4", [128, 4, 32], F32)
    Macc = fw.sb("Macc", [128, 32], F32)
    msum = fw.sb("msum", [128, 32], F32)
    RC4 = fw.sb("RC4", [128, 4, 32], F32)
    tmp4 = fw.sb("tmp4", [128, 4, 32], F32)
    dstf4 = fw.sb("dstf4", [128, 4, 2], F32)
    fw.op("dve", lambda e: e.memset(Macc[:], 0.0), writes=[Macc])

    def gen_front(g):
            b, c = g // 4, g % 4
            aT_ = aTc[0]
            fw.dma("sp", xTc, xT_d, lambda e, g=g: e.dma_start(out=xTc[:], in_=xT_d.t[:, :, g * 512:(g + 1) * 512]))
            fw.dma("sp", aT_, attnT_d, lambda e, aT_=aT_, g=g: e.dma_start(out=aT_[:], in_=attnT_d.t[:, :, g * 512:(g + 1) * 512]))
            for jj in range(4):
                fw.dma("sp", xs4t[jj], x_d, lambda e, b=b, c=c, jj=jj: e.dma_start(
                    out=xs4[:, jj, :], in_=x_d.t[b, c * 512 + jj * 128:c * 512 + (jj + 1) * 128, :]))
            yield
            uc = u1
            if c == 0:
                fw.op("pool", lambda e, uc=uc: e.memset(uc[:, :, 0:2], 0.0), writes=[uc])
            for cc in range(4):
                QQ = (Q0, Q1, Q2, Q3)
                TIn, TC, TBb = QQ[(3 * cc) % 4], QQ[(3 * cc + 1) % 4], QQ[(3 * cc + 2) % 4]
                in_ap, c_ap, b_ap = QAP[TIn], QAP[TC], QAP[TBb]
                cin, ycv = cin2[cc % 2], ycv2[cc % 2]

                for (Tb, dst, col0) in ((TIn, in_ap, cc * 128), (TC, c_ap, 1024 + cc * 128), (TBb, b_ap, 512 + cc * 128)):
                    def mmc(e, dst=dst, col0=col0):
                        for dc in range(8):
                            ins = e.matmul(dst, lhsT=wfm[:, dc, col0:col0 + 128], rhs=xTc[:, dc, :], start=(dc == 0), stop=(dc == 7))
                        return ins
                    fw.op("pe", mmc, reads=[wfm, xTc], writes=[Tb])
                fw.op("act", lambda e, cin=cin, in_ap=in_ap: e.activation(out=cin[:], in_=in_ap, func=AF.Copy), reads=[TIn], writes=[cin])
                fw.op("dve", lambda e, cc=cc, uc=uc, cin=cin, c_ap=c_ap: e.tensor_tensor(out=uc[:, cc, 2:514], in0=c_ap, in1=cin[:], op=ALU.mult),
                      reads=[TC, cin], writes=[uc])
                fw.op("act", lambda e, cc=cc, uc=uc, ycv=ycv: e.activation(out=ycv[:], in_=uc[:, cc, 2:514], func=AF.Identity, scale=cw[:, cc, 2:3]),
                      reads=[uc, cw], writes=[ycv])
                fw.op("dve", lambda e, cc=cc, uc=uc, ycv=ycv: e.scalar_tensor_tensor(out=ycv[:], in0=uc[:, cc, 1:513], scalar=cw[:, cc, 1:2], in1=ycv[:],
                                                                                     op0=ALU.mult, op1=ALU.add), reads=[uc, cw, ycv], writes=[ycv])
                fw.op("dve", lambda e, cc=cc, uc=uc, ycv=ycv: e.scalar_tensor_tensor(out=ycv[:], in0=uc[:, cc, 0:512], scalar=cw[:, cc, 0:1], in1=ycv[:],
                                                                                     op0=ALU.mult, op1=ALU.add), reads=[uc, cw, ycv], writes=[ycv])
                fw.op("dve", lambda e, cc=cc, ycv=ycv, b_ap=b_ap: e.tensor_tensor(out=convT[:, cc, :], in0=b_ap, in1=ycv[:], op=ALU.mult),
                      reads=[TBb, ycv], writes=[convT])
                yield
            yield
            if c < 3:
                fw.op("pool", lambda e, uc=uc: e.tensor_copy(out=uc[:, :, 0:2], in_=uc[:, :, 512:514]), reads=[uc], writes=[uc])
            yield
            for dmc in range(8):
                TGA, TGC, TA, TB = Q0, Q1, Q2, Q3
                A_ap, B_ap, GA_ap, GC_ap = QAP[TA], QAP[TB], QAP[TGA], QAP[TGC]
                gA, gC, m1, m2 = gA2[dmc % 2], gC2[dmc % 2], m12[dmc % 2], m22[dmc % 2]

                for (Tb, dst, col0) in ((TGA, GA_ap, 1536 + dmc * 128), (TGC, GC_ap, 2560 + dmc * 128)):
                    def mmg(e, dst=dst, col0=col0):
                        for dc in range(8):
                            ins = e.matmul(dst, lhsT=wfm[:, dc, col0:col0 + 128], rhs=xTc[:, dc, :], start=(dc == 0), stop=(dc == 7))
                        return ins
                    fw.op("pe", mmg, reads=[wfm, xTc], writes=[Tb])
                fw.op("act", lambda e, dmc=dmc, gA=gA, GA_ap=GA_ap: e.activation(out=gA[:], in_=GA_ap, func=AF.Sigmoid, bias=gb[:, dmc:dmc + 1], scale=1.0),
                      reads=[TGA, gb], writes=[gA])
                fw.op("act", lambda e, dmc=dmc, gC=gC, GC_ap=GC_ap: e.activation(out=gC[:], in_=GC_ap, func=AF.Sigmoid, bias=gb[:, 8 + dmc:9 + dmc], scale=1.0),
                      reads=[TGC, gb], writes=[gC])

                def mmua(e, dmc=dmc, aT_=aT_, A_ap=A_ap):
                    for hc in range(4):
                        ins = e.matmul(A_ap, lhsT=waup[:, hc, dmc * 128:(dmc + 1) * 128], rhs=aT_[:, hc, :], start=(hc == 0), stop=(hc == 3))
                    return ins
                fw.op("pe", mmua, reads=[waup, aT_], writes=[TA])

                def mmub(e, dmc=dmc, B_ap=B_ap):
                    for cc in range(4):
                        ins = e.matmul(B_ap, lhsT=wcup[:, cc, dmc * 128:(dmc + 1) * 128], rhs=convT[:, cc, :], start=(cc == 0), stop=(cc == 3))
                    return ins
                fw.op("pe", mmub, reads=[wcup, convT], writes=[TB])
                fw.op("dve", lambda e, m1=m1, gA=gA, A_ap=A_ap: e.tensor_tensor(out=m1[:], in0=A_ap, in1=gA[:], op=ALU.mult), reads=[TA, gA], writes=[m1])
                fw.op("dve", lambda e, m2=m2, gC=gC, B_ap=B_ap: e.tensor_tensor(out=m2[:], in0=B_ap, in1=gC[:], op=ALU.mult), reads=[TB, gC], writes=[m2])
                fw.op("pool", lambda e, dmc=dmc, m1=m1, m2=m2: e.tensor_tensor(out=mergedT[:, dmc, :], in0=m1[:], in1=m2[:], op=ALU.add),
                      reads=[m1, m2], writes=[mergedT])
                yield
            yield

    def gen_back(g):
            AXX = mybir.AxisListType.X
            mixP = [([Q0, Q1], P01[:, 0:512], P01[:, 512:1024]), ([Q2, Q3], P23[:, 0:512], P23[:, 512:1024]),
                    ([P4, P5], P4[:, 0:512], P5[:, 0:512]), ([P6, P7], P6[:, 0:512], P7[:, 0:512])]
            yield
            for jj in range(4):
                Ts, lo_ap, hi_ap = mixP[jj]

                def mmo(e, jj=jj, lo_ap=lo_ap, hi_ap=hi_ap):
                    for dst, hf in ((lo_ap, 0), (hi_ap, 1)):
                        for dmc in range(8):
                            ins = e.matmul(dst, lhsT=mergedT[:, dmc, jj * 128:(jj + 1) * 128],
                                           rhs=wout[:, dmc, hf * 512:(hf + 1) * 512], start=(dmc == 0), stop=(dmc == 7))
                    return ins
                fw.op("pe", mmo, reads=[mergedT, wout], writes=Ts)
            yield
            for jj in range(4):
                Ts, lo_ap, hi_ap = mixP[jj]
                y_ = ys[jj]
                for hf, ap in ((0, lo_ap), (1, hi_ap)):
                    fw.op("dve", lambda e, y_=y_, jj=jj, hf=hf, ap=ap: e.scalar_tensor_tensor(
                        out=y_[:, hf * 512:(hf + 1) * 512], in0=xs4[:, jj, hf * 512:(hf + 1) * 512], scalar=ALPHA, in1=ap,
                        op0=ALU.mult, op1=ALU.add), reads=[xs4t[jj]] + Ts, writes=[y_])
            yield
            for jj in range(4):
                y_ = ys[jj]
                for hf in range(2):
                    fw.op("dve", lambda e, hf=hf, y_=y_, jj=jj: e.bn_stats(out=st64[:, jj, hf, :], in_=y_[:, hf * 512:(hf + 1) * 512]),
                          reads=[y_], writes=[st64])
            yield
            for jj in range(4):
                fw.op("dve", lambda e, jj=jj: e.bn_aggr(out=mv4[:, jj, :], in_=st64[:, jj, :, :].rearrange("p a s -> p (a s)")),
                      reads=[st64], writes=[mv4])
            fw.op("act", lambda e: e.activation(out=sd4[:], in_=mv4[:, :, 1], func=AF.Sqrt, bias=epsb[:, 0:1], scale=1.0),
                  reads=[mv4, epsb], writes=[sd4])
            fw.op("dve", lambda e: e.reciprocal(out=rstd4[:], in_=sd4[:]), reads=[sd4], writes=[rstd4])
            fw.op("dve", lambda e: e.scalar_tensor_tensor(out=nmr4[:], in0=mv4[:, :, 0], scalar=-1.0, in1=rstd4[:], op0=ALU.mult, op1=ALU.mult),
                  reads=[mv4, rstd4], writes=[nmr4])
            yield
            for jj in range(4):
                y_ = ys[jj]
                fw.op("act", lambda e, y_=y_, jj=jj: e.activation(out=y_[:], in_=y_[:], func=AF.Identity, bias=nmr4[:, jj:jj + 1], scale=rstd4[:, jj:jj + 1]),
                      reads=[y_, nmr4, rstd4], writes=[y_])
            yield
            for jj in range(4):
                y_ = ys[jj]
                fw.op("dve", lambda e, y_=y_: e.tensor_tensor(out=y_[:], in0=y_[:], in1=ln1g[:], op=ALU.mult), reads=[y_, ln1g], writes=[y_])
                fw.op("pool", lambda e, y_=y_: e.tensor_tensor(out=y_[:], in0=y_[:], in1=ln1b[:], op=ALU.add), reads=[y_, ln1b], writes=[y_])
            yield
            for jj in range(4):
                y_ = ys[jj]
                ti = g * 4 + jj
                fw.dma("sp", h_d, y_, lambda e, y_=y_, ti=ti: e.dma_start(out=h_d.t[ti * 128:(ti + 1) * 128, :], in_=y_[:]), nowaw=True, lane=jj)
                fw.op("act", lambda e, y_=y_, jj=jj: e.activation(out=hbf[jj][:], in_=y_[:], func=AF.Copy), reads=[y_], writes=[hbf[jj]])
            yield
            for jj in range(4):
                y_ = ys[jj]
                hT_ = hT2[0]

                def trh(e, y_=y_):
                    for dc in range(8):
                        Pq = P4 if dc < 4 else P5
                        ins = e.transpose(Pq[:, (dc % 4) * 128:(dc % 4 + 1) * 128], y_[:, dc * 128:(dc + 1) * 128], IDF())
                    return ins
                fw.op("pe", trh, reads=[y_, consts], writes=[P4, P5])
                fw.op("act", lambda e, hT_=hT_: e.activation(out=hT_[:, 0:4, :], in_=P4[:].rearrange("p (c t) -> p c t", t=128), func=AF.Copy),
                      reads=[P4], writes=[hT_])
                fw.op("act", lambda e, hT_=hT_: e.activation(out=hT_[:, 4:8, :], in_=P5[:].rearrange("p (c t) -> p c t", t=128), func=AF.Copy),
                      reads=[P5], writes=[hT_])

                def mmr(e, hT_=hT_, jj=jj):
                    for dc in range(8):
                        ins = e.matmul(P6[:, jj * 64:jj * 64 + 36], lhsT=hT_[:, dc, :], rhs=wr[:, dc, :], start=(dc == 0), stop=(dc == 7))
                    return ins
                fw.op("pe", mmr, reads=[hT_, wr], writes=[P6])
            yield
            V = "dve"
            lgG = lg4[:, :, 0:4]
            fw.op(V, lambda e: e.tensor_tensor(out=lg4[:], in0=P6[:, 0:256].rearrange("p (t n) -> p t n", n=64)[:, :, 0:36],
                                               in1=rb[:].unsqueeze(1).to_broadcast([128, 4, 36]), op=ALU.add), reads=[P6, rb], writes=[lg4])
            fw.op(V, lambda e: e.tensor_reduce(out=gmx4[:], in_=lgG, axis=AXX, op=ALU.max), reads=[lg4], writes=[gmx4])
            fw.op(V, lambda e: e.tensor_tensor(out=ohg4[:], in0=lgG, in1=gmx4[:].unsqueeze(2).to_broadcast([128, 4, 4]), op=ALU.is_ge),
                  reads=[lg4, gmx4], writes=[ohg4])
            fw.op(V, lambda e: e.tensor_tensor(out=sh4[:], in0=lgG, in1=gmx4[:].unsqueeze(2).to_broadcast([128, 4, 4]), op=ALU.subtract),
                  reads=[lg4, gmx4], writes=[sh4])
            fw.op("act", lambda e: e.activation(out=sh4[:], in_=sh4[:], func=AF.Exp), reads=[sh4], writes=[sh4])
            fw.op(V, lambda e: e.tensor_reduce(out=seg4[:], in_=sh4[:], axis=AXX, op=ALU.add), reads=[sh4], writes=[seg4])
            fw.op(V, lambda e: e.reciprocal(out=pgrp4[:], in_=seg4[:]), reads=[seg4], writes=[pgrp4])
            fw.op(V, lambda e: e.tensor_tensor(out=prod4[:], in0=lg4[:, :, 4:36].rearrange("p t (g j) -> p t g j", j=8),
                                               in1=ohg4[:].unsqueeze(3).to_broadcast([128, 4, 4, 8]), op=ALU.mult), reads=[lg4, ohg4], writes=[prod4])
            fw.op(V, lambda e: e.tensor_reduce(out=esel4[:], in_=prod4[:].rearrange("p t g j -> p t j g"), axis=AXX, op=ALU.add),
                  reads=[prod4], writes=[esel4])
            fw.op(V, lambda e: e.tensor_reduce(out=mx14[:], in_=esel4[:], axis=AXX, op=ALU.max), reads=[esel4], writes=[mx14])
            fw.op(V, lambda e: e.tensor_tensor(out=oh14[:], in0=esel4[:], in1=mx14[:].unsqueeze(2).to_broadcast([128, 4, 8]), op=ALU.is_ge),
                  reads=[esel4, mx14], writes=[oh14])
            fw.op(V, lambda e: e.scalar_tensor_tensor(out=e24[:].rearrange("p t j -> p (t j)"), in0=oh14[:].rearrange("p t j -> p (t j)"), scalar=NEG,
                                                      in1=esel4[:].rearrange("p t j -> p (t j)"), op0=ALU.mult, op1=ALU.add),
                  reads=[oh14, esel4], writes=[e24])
            fw.op(V, lambda e: e.tensor_reduce(out=mx24[:], in_=e24[:], axis=AXX, op=ALU.max), reads=[e24], writes=[mx24])
            fw.op(V, lambda e: e.tensor_tensor(out=oh24[:], in0=e24[:], in1=mx24[:].unsqueeze(2).to_broadcast([128, 4, 8]), op=ALU.is_ge),
                  reads=[e24, mx24], writes=[oh24])
            fw.op(V, lambda e: e.tensor_tensor(out=ed4[:], in0=mx24[:], in1=mx14[:], op=ALU.subtract), reads=[mx14, mx24], writes=[ed4])
            fw.op("act", lambda e: e.activation(out=ed4[:], in_=ed4[:], func=AF.Exp), reads=[ed4], writes=[ed4])
            fw.op(V, lambda e: e.tensor_scalar(out=w14[:], in0=ed4[:], scalar1=1.0, scalar2=None, op0=ALU.add), reads=[ed4], writes=[w14])
            fw.op(V, lambda e: e.reciprocal(out=w14[:], in_=w14[:]), reads=[w14], writes=[w14])
            fw.op(V, lambda e, g=g: e.tensor_tensor(out=gate_all[:, g * 4:(g + 1) * 4, 0], in0=w14[:], in1=pgrp4[:], op=ALU.mult),
                  reads=[w14, pgrp4], writes=[gate_all])
            fw.op(V, lambda e, g=g: e.tensor_tensor(out=gate_all[:, g * 4:(g + 1) * 4, 1], in0=gate_all[:, g * 4:(g + 1) * 4, 0], in1=ed4[:], op=ALU.mult),
                  reads=[ed4, gate_all], writes=[gate_all])
            for Ek, ohk in ((E14, oh14), (E24, oh24)):
                fw.op(V, lambda e, Ek=Ek, ohk=ohk: e.tensor_tensor(out=Ek[:], in0=ohg4[:].unsqueeze(3).to_broadcast([128, 4, 4, 8]),
                                                                  in1=ohk[:].unsqueeze(2).to_broadcast([128, 4, 4, 8]), op=ALU.mult),
                      reads=[ohg4, ohk], writes=[Ek])
            fw.op(V, lambda e: e.tensor_tensor(out=Mm4[:], in0=E14[:].rearrange("p t g j -> p t (g j)"), in1=E24[:].rearrange("p t g j -> p t (g j)"), op=ALU.add),
                  reads=[E14, E24], writes=[Mm4])

            yield
            def mmrank(e):
                for jj in range(4):
                    dst = P7[:, jj * 32:(jj + 1) * 32]
                    e.matmul(dst, lhsT=TRI(), rhs=Mm4[:, jj, :], start=True, stop=False)
                    for i in range(jj):
                        e.matmul(dst, lhsT=ones_f[:], rhs=Mm4[:, i, :], start=False, stop=False)
                    ins = e.matmul(dst, lhsT=ones_f[:], rhs=Macc[:], start=False, stop=True)
                return ins
            fw.op("pe", mmrank, reads=[consts, ones_f, Mm4, Macc], writes=[P7])
            fw.op(V, lambda e: e.tensor_scalar(out=RC4[:], in0=P7[:, 0:128].rearrange("p (t n) -> p t n", n=32), scalar1=float(CAP - 1), scalar2=None, op0=ALU.min),
                  reads=[P7], writes=[RC4])
            fw.op(V, lambda e: e.tensor_tensor(out=RC4[:], in0=RC4[:], in1=ECAP().unsqueeze(1).to_broadcast([128, 4, 32]), op=ALU.add),
                  reads=[RC4, consts], writes=[RC4])
            fw.op(V, lambda e: e.tensor_reduce(out=msum[:], in_=Mm4[:].rearrange("p t n -> p n t"), axis=AXX, op=ALU.add), reads=[Mm4], writes=[msum])
            fw.op(V, lambda e: e.tensor_tensor(out=Macc[:], in0=Macc[:], in1=msum[:], op=ALU.add), reads=[Macc, msum], writes=[Macc])
            yield
            for k_i, Ek in ((0, E14), (1, E24)):
                fw.op(V, lambda e, Ek=Ek: e.tensor_tensor(out=tmp4[:], in0=Ek[:].rearrange("p t g j -> p t (g j)"), in1=RC4[:], op=ALU.mult),
                      reads=[Ek, RC4], writes=[tmp4])
                fw.op(V, lambda e, k_i=k_i: e.tensor_reduce(out=dstf4[:, :, k_i], in_=tmp4[:], axis=AXX, op=ALU.add), reads=[tmp4], writes=[dstf4])
            fw.op(V, lambda e, g=g: e.tensor_copy(out=dest_all[:, g * 4:(g + 1) * 4, :], in_=dstf4[:]), reads=[dstf4], writes=[dest_all])
            yield
            for jj in range(4):
                ti = g * 4 + jj
                for k_i in range(2):
                    fw.dma("pool", xs_d, hbf[jj], lambda e, jj=jj, ti=ti, k_i=k_i: e.indirect_dma_start(
                        out=xs_d.t[:, :], out_offset=bass.IndirectOffsetOnAxis(ap=dest_all[:, ti, k_i:k_i + 1], axis=0),
                        in_=hbf[jj][:, :], in_offset=None), extra_reads=[dest_all], nowaw=(ti > 0 or k_i > 0), lane="s%d" % (jj * 2 + k_i))


            yield

    run(gen_front(0))
    for g in range(8):
        gbk = gen_back(g)
        for _ in range(3):
            next(gbk)
        if g + 1 < 8:
            interleave(gbk, 11, gen_front(g + 1), 18)
        else:
            run(gbk)

    if stop_after == "B":
        fw.wait_all("sp", [h_d, xs_d])
        fw.finish()
        return nc

    fw.barrier()
    fw.release(base_mark)

    wgb = [fw.sb(f"wgb{i}", [128, 8, 512], BF16) for i in range(3)]
    wub = [fw.sb(f"wub{i}", [128, 8, 512], BF16) for i in range(3)]
    wdb = [fw.sb(f"wdb{i}", [128, 4, D], BF16) for i in range(4)]
    xsb = [fw.sb(f"xsb{i}", [128, 3, D], BF16) for i in range(3)]
    xsT = [fw.sb(f"xsT{i}", [128, 8, CAP], BF16) for i in range(2)]
    sg = [fw.sb(f"sg{i}", [128, CAP], F32) for i in range(2)]
    hidT = [fw.sb(f"hidT{i}", [128, 4, CAP], BF16) for i in range(2)]
    ysb = [fw.sb(f"ysb{i}", [128, D], F32) for i in range(3)]
    ycn = [0]

    def moe_load(ex):
        wg_, wu_, wd_, xs_ = wgb[ex % 3], wub[ex % 3], wdb[ex % 4], xsb[ex % 3]
        fw.dma("sp", xs_, xs_d, lambda e, xs_=xs_, ex=ex: e.dma_start(
            out=xs_[:], in_=xs_d.t[ex * CAP:(ex + 1) * CAP, :].rearrange("(s p) d -> p s d", p=128)))
        fw.dma("sp", wg_, wgc_d, lambda e, wg_=wg_, ex=ex: e.dma_start(out=wg_[:], in_=wgc_d.t[ex].rearrange("(c p) n -> p c n", p=128)))
        fw.dma("sp", wu_, wuc_d, lambda e, wu_=wu_, ex=ex: e.dma_start(out=wu_[:], in_=wuc_d.t[ex].rearrange("(c p) n -> p c n", p=128)))
        fw.dma("sp", wd_, wdc_d, lambda e, wd_=wd_, ex=ex: e.dma_start(out=wd_[:], in_=wdc_d.t[ex].rearrange("(c p) n -> p c n", p=128)))

    def moe_T(ex):
        xs_, xT_ = xsb[ex % 3], xsT[ex % 2]
        for st in range(3):
            yield
            Pm = (P5, P6)[nxt("m", 2)]

            def trs(e, Pm=Pm, xs_=xs_, st=st):
                for dc in range(8):
                    ins = e.transpose(bview(Pm)[:, dc, :], xs_[:, st, dc * 128:(dc + 1) * 128], ident_b[:])
                return ins
            fw.op("pe", trs, reads=[xs_, ident_b], writes=[Pm])
            if st == 1:
                fw.op("dve", lambda e, Pm=Pm, xT_=xT_, st=st: e.tensor_copy(out=xT_[:, :, st * 128:(st + 1) * 128], in_=bview(Pm)),
                      reads=[Pm], writes=[xT_])
            else:
                fw.op("act", lambda e, Pm=Pm, xT_=xT_, st=st: e.activation(out=xT_[:, :, st * 128:(st + 1) * 128], in_=bview(Pm), func=AF.Copy),
                      reads=[Pm], writes=[xT_])

    def moe_GU(ex):
        wg_, wu_ = wgb[ex % 3], wub[ex % 3]
        xT_, hid_ = xsT[ex % 2], hidT[ex % 2]
        for fc in range(4):
            yield
            Pgu = (P01, P23)[nxt("pab", 2)]
            sg_ = sg[fc % 2]

            def mmgu(e, Pgu=Pgu, fc=fc, wg_=wg_, wu_=wu_, xT_=xT_):
                for dc in range(8):
                    e.matmul(Pgu[:, 0:CAP], lhsT=wg_[:, dc, fc * 128:(fc + 1) * 128], rhs=xT_[:, dc, :], start=(dc == 0), stop=(dc == 7))
                for dc in range(8):
                    ins = e.matmul(Pgu[:, 512:512 + CAP], lhsT=wu_[:, dc, fc * 128:(fc + 1) * 128], rhs=xT_[:, dc, :], start=(dc == 0), stop=(dc == 7))
                return ins
            fw.op("pe", mmgu, reads=[wg_, wu_, xT_], writes=[Pgu])
            fw.op("act", lambda e, Pgu=Pgu, sg_=sg_: e.activation(out=sg_[:], in_=Pgu[:, 0:CAP], func=AF.Silu), reads=[Pgu], writes=[sg_])
            fw.op("dve", lambda e, Pgu=Pgu, sg_=sg_, hid_=hid_, fc=fc: e.tensor_tensor(out=hid_[:, fc, :], in0=Pgu[:, 512:512 + CAP], in1=sg_[:], op=ALU.mult),
                  reads=[Pgu, sg_], writes=[hid_])

    def moe_DN(ex):
        wd_, hid_ = wdb[ex % 4], hidT[ex % 2]
        for st in range(3):
            yield
            yc = ycn[0]
            ycn[0] += 1
            y_ = ysb[yc % 3]

            def mmdn(e, hid_=hid_, wd_=wd_, st=st):
                for hf, Py in ((0, P4), (1, P7)):
                    for fc in range(4):
                        ins = e.matmul(Py[:, 0:512], lhsT=hid_[:, fc, st * 128:(st + 1) * 128], rhs=wd_[:, fc, hf * 512:(hf + 1) * 512],
                                       start=(fc == 0), stop=(fc == 3))
                return ins
            fw.op("pe", mmdn, reads=[hid_, wd_], writes=[P4, P7])
            fw.op("act", lambda e, y_=y_: e.activation(out=y_[:, 0:512], in_=P4[:, 0:512], func=AF.Copy), reads=[P4], writes=[y_])
            fw.op("dve", lambda e, y_=y_: e.tensor_copy(out=y_[:, 512:1024], in_=P7[:, 0:512]), reads=[P7], writes=[y_])
            r0 = ex * CAP + st * 128
            fw.dma("sp", y_d, y_, lambda e, y_=y_, r0=r0: e.dma_start(out=y_d.t[r0:r0 + 128, :], in_=y_[:]), nowaw=True, lane=yc % 3)

    def step(gen):
        if gen is not None:
            next(gen, None)

    moe_load(0)
    moe_load(1)
    for _ in moe_T(0):
        pass
    for ex in range(NEXP + 1):
        if ex + 2 < NEXP:
            moe_load(ex + 2)
        gu = moe_GU(ex) if ex < NEXP else None
        tr_ = moe_T(ex + 1) if ex + 1 < NEXP else None
        dn = moe_DN(ex - 1) if ex >= 1 else None
        for g_ in (gu, tr_, dn):
            step(g_)
        for k in range(4):
            step(gu)
            if k < 3:
                step(tr_)
                step(dn)
        for g_ in (gu, tr_, dn):
            if g_ is not None:
                for _ in g_:
                    pass

    fw.barrier()
    fw.release(base_mark)

    ln2g = fw.sb("ln2g", [128, D], F32)
    ln2b = fw.sb("ln2b", [128, D], F32)
    fw.dma("sp", ln2g, lnp_d, lambda e: e.dma_start(out=ln2g[:], in_=lnp_d.t[2:3, :].partition_broadcast(128)))
    fw.dma("sp", ln2b, lnp_d, lambda e: e.dma_start(out=ln2b[:], in_=lnp_d.t[3:4, :].partition_broadcast(128)))
    NBF = 8
    Y0 = [fw.sb(f"Y0_{i}", [128, D], F32) for i in range(NBF)]
    Y1 = [fw.sb(f"Y1_{i}", [128, D], F32) for i in range(NBF)]
    hr = [fw.sb(f"hr{i}", [128, D], F32) for i in range(NBF)]
    zz = [fw.sb(f"zz{i}", [128, D], F32) for i in range(NBF)]
    s1f = fw.sb("s1f", [128, 4], F32)
    s2f = fw.sb("s2f", [128, 4], F32)
    junkF = fw.sb("junkF", [128, D], BF16)
    mvf = fw.sb("mvf", [128, 4, 2], F32)
    sdf = fw.sb("sdf", [128, 4], F32)
    rstdf = fw.sb("rstdf", [128, 4], F32)
    nmrf = fw.sb("nmrf", [128, 4], F32)
    epsb2 = fw.sb("epsbb", [128, 1], F32)
    fw.op("dve", lambda e: e.memset(epsb2[:], LN_EPS), writes=[epsb2])

    def fin_load(ti):
        a0, a1, h_ = Y0[ti % NBF], Y1[ti % NBF], hr[ti % NBF]
        fw.dma("pool", a0, y_d, lambda e, a0=a0, ti=ti: e.indirect_dma_start(
            out=a0[:, :], out_offset=None, in_=y_d.t[:, :],
            in_offset=bass.IndirectOffsetOnAxis(ap=dest_all[:, ti, 0:1], axis=0)), extra_reads=[dest_all])
        fw.dma("pool", a1, y_d, lambda e, a1=a1, ti=ti: e.indirect_dma_start(
            out=a1[:, :], out_offset=None, in_=y_d.t[:, :],
            in_offset=bass.IndirectOffsetOnAxis(ap=dest_all[:, ti, 1:2], axis=0)), extra_reads=[dest_all])
        fw.dma("sp", h_, h_d, lambda e, h_=h_, ti=ti: e.dma_start(out=h_[:], in_=h_d.t[ti * 128:(ti + 1) * 128, :]))

    for ti in range(4):
        fin_load(ti)
    for grp in range(8):
        tis = list(range(grp * 4, grp * 4 + 4))
        if grp + 1 < 8:
            for ti in range(grp * 4 + 4, grp * 4 + 8):
                fin_load(ti)
        finP = [([P01], P01[:, 0:512], P01[:, 512:1024]), ([P23], P23[:, 0:512], P23[:, 512:1024]),
                ([P4, P5], P4[:, 0:512], P5[:, 0:512]), ([P6, P7], P6[:, 0:512], P7[:, 0:512])]
        for k, ti in enumerate(tis):
            a0 = Y0[ti % NBF]
            Ts, lo_ap, hi_ap = finP[k]
            for hf, ap in ((0, lo_ap), (1, hi_ap)):
                fw.op("act", lambda e, a0=a0, ap=ap, hf=hf, ti=ti: e.activation(out=ap, in_=a0[:, hf * 512:(hf + 1) * 512], func=AF.Identity,
                                                                              scale=gate_all[:, ti, 0:1]), reads=[a0, gate_all], writes=Ts)
        for k, ti in enumerate(tis):
            a1, h_, z_ = Y1[ti % NBF], hr[ti % NBF], zz[ti % NBF]
            Ts, lo_ap, hi_ap = finP[k]
            for hf, ap in ((0, lo_ap), (1, hi_ap)):
                fw.op("dve", lambda e, a1=a1, ap=ap, hf=hf, ti=ti: e.scalar_tensor_tensor(
                    out=ap, in0=a1[:, hf * 512:(hf + 1) * 512], scalar=gate_all[:, ti, 1:2], in1=ap,
                    op0=ALU.mult, op1=ALU.add), reads=[a1, gate_all] + Ts, writes=Ts)
            for hf, ap in ((0, lo_ap), (1, hi_ap)):
                fw.op("dve", lambda e, h_=h_, z_=z_, ap=ap, hf=hf: e.scalar_tensor_tensor(
                    out=z_[:, hf * 512:(hf + 1) * 512], in0=h_[:, hf * 512:(hf + 1) * 512], scalar=ALPHA, in1=ap,
                    op0=ALU.mult, op1=ALU.add), reads=[h_] + Ts, writes=[z_])
        for k, ti in enumerate(tis):
            z_ = zz[ti % NBF]
            fw.op("act", lambda e, z_=z_, k=k: e.activation(out=junkF[:], in_=z_[:], func=AF.Identity, accum_out=s1f[:, k:k + 1]),
                  reads=[z_], writes=[s1f, junkF])
            fw.op("act", lambda e, z_=z_, k=k: e.activation(out=junkF[:], in_=z_[:], func=AF.Square, accum_out=s2f[:, k:k + 1]),
                  reads=[z_], writes=[s2f, junkF])
        fw.op("dve", lambda e: e.tensor_scalar(out=mvf[:, :, 0], in0=s1f[:], scalar1=1.0 / D, scalar2=None, op0=ALU.mult), reads=[s1f], writes=[mvf])
        fw.op("dve", lambda e: e.tensor_tensor(out=mvf[:, :, 1], in0=mvf[:, :, 0], in1=mvf[:, :, 0], op=ALU.mult), reads=[mvf], writes=[mvf])
        fw.op("dve", lambda e: e.scalar_tensor_tensor(out=mvf[:, :, 1], in0=s2f[:], scalar=1.0 / D, in1=mvf[:, :, 1], op0=ALU.mult, op1=ALU.subtract),
              reads=[s2f, mvf], writes=[mvf])
        fw.op("act", lambda e: e.activation(out=sdf[:], in_=mvf[:, :, 1], func=AF.Sqrt, bias=epsb2[:, 0:1], scale=1.0), reads=[mvf, epsb2], writes=[sdf])
        fw.op("dve", lambda e: e.reciprocal(out=rstdf[:], in_=sdf[:]), reads=[sdf], writes=[rstdf])
        fw.op("dve", lambda e: e.scalar_tensor_tensor(out=nmrf[:], in0=mvf[:, :, 0], scalar=-1.0, in1=rstdf[:], op0=ALU.mult, op1=ALU.mult),
              reads=[mvf, rstdf], writes=[nmrf])
        for k, ti in enumerate(tis):
            z_ = zz[ti % NBF]
            Ts, lo_ap, hi_ap = finP[k]
            for hf, ap in ((0, lo_ap), (1, hi_ap)):
                fw.op("act", lambda e, z_=z_, k=k, ap=ap, hf=hf: e.activation(out=ap, in_=z_[:, hf * 512:(hf + 1) * 512], func=AF.Identity,
                                                                             bias=nmrf[:, k:k + 1], scale=rstdf[:, k:k + 1]),
                      reads=[z_, nmrf, rstdf], writes=Ts)
        for k, ti in enumerate(tis):
            z_ = zz[ti % NBF]
            Ts, lo_ap, hi_ap = finP[k]
            for hf, ap in ((0, lo_ap), (1, hi_ap)):
                fw.op("dve", lambda e, z_=z_, ap=ap, hf=hf: e.tensor_tensor(out=z_[:, hf * 512:(hf + 1) * 512], in0=ap,
                                                                         in1=ln2g[:, hf * 512:(hf + 1) * 512], op=ALU.mult),
                      reads=Ts + [ln2g], writes=[z_])
            fw.op("pool", lambda e, z_=z_: e.tensor_tensor(out=z_[:], in0=z_[:], in1=ln2b[:], op=ALU.add), reads=[z_, ln2b], writes=[z_])
            bb, tq = ti // 16, ti % 16
            fw.dma("sp", out_d, z_, lambda e, z_=z_, bb=bb, tq=tq: e.dma_start(out=out_d.t[bb, tq * 128:(tq + 1) * 128, :], in_=z_[:]),
                   nowaw=True, lane=ti % NBF)
    fw.wait_all("sp", [out_d])
    fw.finish()
    return nc


def _rope_tables():
    half = 8
    inv_freq = (np.float32(500000.0) ** (-(np.arange(half, dtype=np.float32) / np.float32(half)))).astype(np.float32)
    ang = (np.arange(S, dtype=np.float32)[:, None] * inv_freq[None, :]).astype(np.float32)
    return np.stack([np.cos(ang), np.sin(ang)]).astype(np.float32)


def _consts():
    c = np.zeros((128, 288), np.float32)
    c[:, 0:128] = np.eye(128, dtype=np.float32)
    c[:, 128:256] = np.triu(np.ones((128, 128), np.float32), k=1)
    c[:, 256:288] = (np.arange(32, dtype=np.float32) * CAP)[None, :]
    return c


def prep_inputs(x, w_in, gate_bias, w_attn_up, w_conv_up, conv_w, w_out, ln1_g, ln1_b,
                router_group_w, router_group_b, router_expert_w, router_expert_b,
                w_gate_e, w_up_e, w_down_e, ln2_g, ln2_b):
    f = lambda a: np.ascontiguousarray(np.asarray(a, dtype=np.float32))
    w = f(w_in)[0]
    w_tm = np.concatenate([w[:, 0:512], w[:, 768:1280], w[:, 512:640], w[:, 1280:1344], w[:, 640:768], w[:, 1344:1352]], axis=1)
    w_fm = w[:, 1352:4936]
    shared = {
        "w_tm": f(w_tm), "w_fm": f(w_fm), "cs": _rope_tables(), "consts": _consts(),
        "gbias": f(f(gate_bias)[0].reshape(16, 128).T),
        "convw": f(f(conv_w)[0].T.reshape(4, 128, 3).transpose(1, 0, 2)),
        "w_aup": f(w_attn_up)[0], "w_cup": f(w_conv_up)[0], "w_out": f(w_out)[0],
        "lnp": f(np.stack([f(ln1_g)[0], f(ln1_b)[0], f(ln2_g)[0], f(ln2_b)[0]])),
        "w_r": f(np.concatenate([f(router_group_w)[0], f(router_expert_w)[0].transpose(1, 0, 2).reshape(D, 32)], axis=1)),
        "b_r": f(np.concatenate([f(router_group_b)[0], f(router_expert_b)[0].reshape(32)])[None, :]),
        "w_gate": f(w_gate_e)[0], "w_up": f(w_up_e)[0], "w_down": f(w_down_e)[0],
        "zeros": np.zeros((128, D), dtype=ml_dtypes.bfloat16),
    }
    xx = f(x)
    return [dict(shared, x=np.ascontiguousarray(xx[2 * i:2 * i + 2])) for i in range(NCORES)]


def kernel(**inputs):
    in_maps = prep_inputs(**inputs)
    nc = build_program()
    res = run_bass_kernel_spmd(nc, in_maps, core_ids=list(range(NCORES)))
    return np.concatenate([np.asarray(r["out"], dtype=np.float32) for r in res.results], axis=0)
```

```python
import math
import numpy as np
import ml_dtypes
import concourse.bass as bass
import concourse.mybir as mybir
from concourse.bass_utils import run_bass_kernel_spmd

F32 = mybir.dt.float32
BF16 = mybir.dt.bfloat16
I32 = mybir.dt.int32
ALU = mybir.AluOpType
AF = mybir.ActivationFunctionType

NCORES = 8
S = 2048
D = 1024
NT = 16
CAP = 384
NEXP = 32
ALPHA = 2.0 ** 0.25
LN_EPS = 1e-5
TOPK = 256
SEARCH_B = 128.0
SEARCH_IT = 19
NEG = -1.0e30

ENGS = ("pe", "act", "dve", "pool", "sp")


class T:
    __slots__ = ("t", "w", "r", "lanes", "name")

    def __init__(self, t, name=""):
        self.t = t
        self.w = {}
        self.r = {}
        self.lanes = {}
        self.name = name

    def __getitem__(self, k):
        return self.t[k]


class FW:
    def __init__(self, nc):
        self.nc = nc
        self.ops = {e: [] for e in ENGS}
        self.esem = {}
        self.ecnt = {e: 0 for e in ENGS}
        self.waited = {e: {} for e in ENGS}
        self.stack = []
        self.dma_ts = []
        for e in ENGS:
            self.esem[e] = nc.alloc_semaphore(name="c_" + e)

    def sb(self, name, shape, dtype):
        g = self.nc.sbuf_tensor("s_" + name, list(shape), dtype)
        tt = T(g.__enter__(), name)
        self.stack.append((tt, g))
        return tt

    def ps(self, name, shape, dtype):
        g = self.nc.psum_tensor("p_" + name, list(shape), dtype)
        tt = T(g.__enter__(), name)
        self.stack.append((tt, g))
        return tt

    def mark(self):
        return len(self.stack)

    def release(self, mark):
        while len(self.stack) > mark:
            tt, g = self.stack.pop()
            g.__exit__(None, None, None)

    def dram(self, name, shape, dtype, kind="Internal"):
        return T(self.nc.dram_tensor(name, list(shape), dtype, kind=kind), name)

    def _waits(self, e, reads, writes, nowaw=False):
        need = {}
        for t in reads:
            for s, v in t.w.items():
                need[s] = max(need.get(s, 0), v)
        for t in writes:
            if not nowaw:
                for s, v in t.w.items():
                    need[s] = max(need.get(s, 0), v)
            for s, v in t.r.items():
                need[s] = max(need.get(s, 0), v)
        out = []
        wd = self.waited[e]
        for s, v in need.items():
            if s is self.esem[e] and e in ("pe", "sp"):
                continue
            if wd.get(s, 0) >= v:
                continue
            wd[s] = v
            out.append((s, v))
        return out

    def op(self, e, fn, reads=(), writes=()):
        ws = self._waits(e, reads, writes)
        self.ecnt[e] += 1
        sem = self.esem[e]
        ev = (sem, self.ecnt[e])

        def run(eng, ws=ws, fn=fn, sem=sem):
            for s, v in ws:
                eng.wait_ge(s, v)
            fn(eng).then_inc(sem, 1)
        self.ops[e].append(run)
        for t in reads:
            t.r[sem] = max(t.r.get(sem, 0), ev[1])
        for t in writes:
            t.w = {sem: ev[1]}
            t.r = {}
        return ev

    def dma(self, q, out_t, in_t, fn, extra_reads=(), nowaw=False, lane=0):
        reads = [in_t] + list(extra_reads)
        ws = self._waits(q, reads, [out_t], nowaw=nowaw)
        if lane not in out_t.lanes:
            out_t.lanes[lane] = [self.nc.alloc_semaphore(name="d_%s_%s" % (out_t.name, lane)), 0]
            if out_t not in self.dma_ts:
                self.dma_ts.append(out_t)
        ln = out_t.lanes[lane]
        ln[1] += 16
        sem = ln[0]
        ev = (sem, ln[1])

        def run(eng, ws=ws, fn=fn, sem=sem):
            for s, v in ws:
                eng.wait_ge(s, v)
            fn(eng).then_inc(sem, 16)
        self.ops[q].append(run)
        for t in reads:
            t.r[sem] = max(t.r.get(sem, 0), ev[1])
        if nowaw:
            out_t.w[sem] = ev[1]
        else:
            out_t.w = {sem: ev[1]}
            out_t.r = {}
        return ev

    def barrier(self):
        evs = [(self.esem[e], self.ecnt[e]) for e in ENGS if self.ecnt[e] > 0]
        for t in self.dma_ts:
            evs += [(ln[0], ln[1]) for ln in t.lanes.values() if ln[1] > 0]
        for e in ENGS:
            ws = []
            wd = self.waited[e]
            for s, v in evs:
                if s is self.esem[e]:
                    continue
                if wd.get(s, 0) >= v:
                    continue
                wd[s] = v
                ws.append((s, v))

            def run(eng, ws=ws):
                for s, v in ws:
                    eng.wait_ge(s, v)
            self.ops[e].append(run)

    def wait_all(self, e, tiles):
        ws = self._waits(e, tiles, [])

        def run(eng, ws=ws):
            for s, v in ws:
                eng.wait_ge(s, v)
        self.ops[e].append(run)

    def finish(self):
        with self.nc.Block() as block:
            @block.tensor
            def _(eng):
                for f in self.ops["pe"]:
                    f(eng)

            @block.scalar
            def _(eng):
                for f in self.ops["act"]:
                    f(eng)

            @block.vector
            def _(eng):
                for f in self.ops["dve"]:
                    f(eng)

            @block.gpsimd
            def _(eng):
                for f in self.ops["pool"]:
                    f(eng)

            @block.sync
            def _(eng):
                for f in self.ops["sp"]:
                    f(eng)
        self.release(0)


def bview(t, n=128):
    return t[:].bitcast(BF16).rearrange("p (c t) -> p c t", t=n)


def build_program(stop_after=None, dbg=False):
    nc = bass.Bass("TRN2", target_bir_lowering=False)
    fw = FW(nc)
    EI = "ExternalInput"
    x_d = fw.dram("x", [2, S, D], F32, EI)
    wtm_d = fw.dram("w_tm", [D, 1352], F32, EI)
    wfm_d = fw.dram("w_fm", [D, 3584], F32, EI)
    cs_d = fw.dram("cs", [2, S, 8], F32, EI)
    const_d = fw.dram("consts", [128, 288], F32, EI)
    gb_d = fw.dram("gbias", [128, 16], F32, EI)
    convw_d = fw.dram("convw", [128, 4, 3], F32, EI)
    waup_d = fw.dram("w_aup", [512, D], F32, EI)
    wcup_d = fw.dram("w_cup", [512, D], F32, EI)
    wout_d = fw.dram("w_out", [D, D], F32, EI)
    lnp_d = fw.dram("lnp", [4, D], F32, EI)
    wr_d = fw.dram("w_r", [D, 36], F32, EI)
    rb_d = fw.dram("b_r", [1, 36], F32, EI)
    if stop_after is None:
        wg_d = fw.dram("w_gate", [NEXP, D, 512], F32, EI)
        wu_d = fw.dram("w_up", [NEXP, D, 512], F32, EI)
        wd_d = fw.dram("w_down", [NEXP, 512, D], F32, EI)
    zeros_d = fw.dram("zeros", [128, D], BF16, EI)
    out_d = fw.dram("out", [2, S, D], F32, "ExternalOutput")
    SK = "ExternalOutput" if dbg else "Internal"
    attnT_d = fw.dram("attnT_scr", [128, 4, 2 * S], BF16, SK)
    h_d = fw.dram("h_scr", [2 * S, D], F32, SK)
    xs_d = fw.dram("xs_scr", [NEXP * CAP, D], BF16, SK)
    y_d = fw.dram("y_scr", [NEXP * CAP, D], F32, SK)
    xT_d = fw.dram("xT_scr", [128, 8, 2 * S], BF16, "Internal")
    wgc_d = fw.dram("wg_bf", [NEXP, D, 512], BF16, "Internal")
    wuc_d = fw.dram("wu_bf", [NEXP, D, 512], BF16, "Internal")
    wdc_d = fw.dram("wd_bf", [NEXP, 512, D], BF16, "Internal")
    if dbg:
        dbg_feat = fw.dram("dbg_feat", [128, 14, S], BF16, "ExternalOutput")
        dbg_v = fw.dram("dbg_v", [128, NT, 2, 65], BF16, "ExternalOutput")
        dbg_wi = fw.dram("dbg_wi", [128, NT, 8], F32, "ExternalOutput")
        dbg_sc = fw.dram("dbg_sc", [NT, 128, S], F32, "ExternalOutput")
        dbg_mk = fw.dram("dbg_mk", [NT, 128, S], BF16, "ExternalOutput")
        dbg_rt = fw.dram("dbg_rt", [128, 32, 8], F32, "ExternalOutput")

    P01 = fw.ps("P01", [128, 1024], F32)
    P23 = fw.ps("P23", [128, 1024], F32)
    P4 = fw.ps("P4", [128, 512], F32)
    P5 = fw.ps("P5", [128, 512], F32)
    P6 = fw.ps("P6", [128, 512], F32)
    P7 = fw.ps("P7", [128, 512], F32)
    Q0, Q1, Q2, Q3 = T(P01.t, "Q0"), T(P01.t, "Q1"), T(P23.t, "Q2"), T(P23.t, "Q3")
    QAP = {Q0: P01[:, 0:512], Q1: P01[:, 512:1024], Q2: P23[:, 0:512], Q3: P23[:, 512:1024]}

    consts = fw.sb("consts", [128, 288], F32)
    fw.dma("sp", consts, const_d, lambda e: e.dma_start(out=consts[:], in_=const_d.t.ap()))
    ident_b = fw.sb("ident_b", [128, 128], BF16)
    fw.op("dve", lambda e: e.tensor_copy(out=ident_b[:], in_=consts[:, 0:128]), reads=[consts], writes=[ident_b])
    ones_f = fw.sb("ones_f", [128, 128], F32)
    fw.op("dve", lambda e: e.memset(ones_f[:], 1.0), writes=[ones_f])
    ident_f = consts

    def IDF():
        return consts[:, 0:128]

    def TRI():
        return consts[:, 128:256]

    def ECAP():
        return consts[:, 256:288]

    dest_all = fw.sb("dest_all", [128, 32, 2], I32)
    gate_all = fw.sb("gate_all", [128, 32, 2], F32)
    base_mark = fw.mark()

    wtm = fw.sb("wtm", [128, 8, 1352], BF16)
    fw.dma("pool", wtm, wtm_d, lambda e: e.dma_start(out=wtm[:], in_=wtm_d.t.ap().rearrange("(c p) n -> p c n", p=128)))
    cs_sb = fw.sb("cs_sb", [128, 2, NT, 8], F32)
    for a in range(2):
        fw.dma("sp", cs_sb, cs_d, lambda e, a=a: e.dma_start(
            out=cs_sb[:, a, :, :], in_=cs_d.t[a].rearrange("(t p) i -> p t i", p=128)))
    xst = [fw.sb(f"xst{i}", [128, D], F32) for i in range(2)]
    xTt = [fw.sb(f"xTt{i}", [128, 8, 128], BF16) for i in range(2)]
    qq = [fw.sb(f"qq{i}", [128, 16, 64], BF16) for i in range(2)]
    kk1 = fw.sb("kkone", [128, 3, 64], BF16)
    kk = [fw.sb(f"kk{i}", [128, 6, 2, 64], BF16) for i in range(2)]
    for _k in kk:
        fw.op("pool", lambda e, _k=_k: e.memset(_k[:], 0.0), writes=[_k])
    rt = [fw.sb(f"rt{i}", [128, 16, 8], F32) for i in range(4)]
    rk = [fw.sb(f"rk{i}", [128, 3, 8], F32) for i in range(4)]
    featT = fw.sb("featT", [128, 14, S], BF16)
    V_aug = fw.sb("V_aug", [128, NT, 2, 65], BF16)
    wi_sb = fw.sb("wi_sb", [128, NT, 8], F32)
    sc = [fw.sb(f"sc{i}", [128, S], F32) for i in range(4)]
    rbuf = [fw.sb(f"rbuf{i}", [128, 2, 512], BF16) for i in range(3)]
    dg = [fw.sb(f"dg{i}", [128, 8, 128], BF16) for i in range(2)]
    mk = [fw.sb(f"mk{i}", [128, S], BF16) for i in range(2)]
    junkD, junkA = mk[0], mk[1]
    cnt4 = fw.sb("cnt4", [128, 4], F32)
    cntT = [T(cnt4.t, f"cnt4_{i}") for i in range(4)]
    fw.op("dve", lambda e: e.memset(cnt4[:], 0.0), writes=cntT)
    g4 = fw.sb("g4", [128, 4], F32)
    d8 = fw.sb("d8", [128, 2, 4], F32)
    st8 = fw.sb("st8", [128, 2, 4], F32)
    thr4 = fw.sb("thr4", [128, 4], F32)
    stT = [T(st8.t, f"st8_{i}") for i in range(2)]
    gT = [T(g4.t, f"g4_{i}") for i in range(2)]
    dT = [T(d8.t, f"d8_{i}") for i in range(2)]
    lo4 = fw.sb("lo4", [128, 4], F32)
    ssT = fw.sb("ssT", [128, SEARCH_IT, 2, 4], F32)
    _stp = SEARCH_B
    for _it in range(SEARCH_IT):
        fw.op("pool", lambda e, _it=_it, _stp=_stp: e.memset(ssT[:, _it, 0, :], float(_stp)), writes=[ssT])
        fw.op("pool", lambda e, _it=_it, _stp=_stp: e.memset(ssT[:, _it, 1, :], float(-_stp)), writes=[ssT])
        _stp = _stp * 0.5
    negb = fw.sb("negb", [128, 1], F32)
    fw.op("pool", lambda e: e.memset(negb[:], -30000.0), writes=[negb])
    mT = [fw.sb(f"mT{i}", [128, NT, 512], BF16) for i in range(2)]
    ebuf = [fw.sb(f"ebuf{i}", [128, 2, 512], BF16) for i in range(3)]
    rec = fw.sb("rec", [128, 4], F32)
    attn_tm = [fw.sb(f"attn_tm{i}", [128, 4, 512], BF16) for i in range(1)]
    attnT_c = [fw.sb(f"attnT_c{i}", [128, 4, 512], BF16) for i in range(2)]

    fw.op("pool", lambda e: e.memset(V_aug[:], 1.0), writes=[V_aug])

    ctr = {"pab": 0, "acc": 0, "r": 0, "m": 0, "e": 0}

    def nxt(key, n):
        v = ctr[key] % n
        ctr[key] += 1
        return v

    def rope(src3, nh, tt, tmps, dst3, srcT):
        cosb = cs_sb[:, 0, tt, :].unsqueeze(1).to_broadcast([128, nh, 8])
        sinb = cs_sb[:, 1, tt, :].unsqueeze(1).to_broadcast([128, nh, 8])
        t1, t2, t3, t4 = tmps
        x1 = src3[:, :, 0:8]
        x2 = src3[:, :, 8:16]
        fw.op("dve", lambda e: e.tensor_tensor(out=t1[:], in0=x1, in1=cosb, op=ALU.mult), reads=[srcT, cs_sb], writes=[t1])
        fw.op("dve", lambda e: e.tensor_tensor(out=t2[:], in0=x2, in1=sinb, op=ALU.mult), reads=[srcT, cs_sb], writes=[t2])
        fw.op("dve", lambda e: e.tensor_tensor(out=t3[:], in0=x2, in1=cosb, op=ALU.mult), reads=[srcT, cs_sb], writes=[t3])
        fw.op("dve", lambda e: e.tensor_tensor(out=t4[:], in0=x1, in1=sinb, op=ALU.mult), reads=[srcT, cs_sb], writes=[t4])
        return (t1, t2, t3, t4)

    dg_done = set()
    for b in range(2):
        def a1_load(tt, b=b):
            xs_ = xst[tt % 2]
            fw.dma("sp", xs_, x_d, lambda e, xs_=xs_, tt=tt, b=b: e.dma_start(out=xs_[:], in_=x_d.t[b, tt * 128:(tt + 1) * 128, :]))

        def a1_tr(tt, b=b):
            xs_ = xst[tt % 2]
            xT = xTt[tt % 2]

            def tr(e, xs_=xs_):
                for dc in range(8):
                    Pq = P5 if dc < 4 else P6
                    i = e.transpose(Pq[:, (dc % 4) * 128:(dc % 4 + 1) * 128], xs_[:, dc * 128:(dc + 1) * 128], IDF())
                return i
            fw.op("pe", tr, reads=[xs_, consts], writes=[P5, P6])
            fw.op("act", lambda e, xT=xT: e.activation(out=xT[:, 0:4, :], in_=P5[:].rearrange("p (c t) -> p c t", t=128), func=AF.Copy),
                  reads=[P5], writes=[xT])
            fw.op("dve", lambda e, xT=xT: e.tensor_copy(out=xT[:, 4:8, :], in_=P6[:].rearrange("p (c t) -> p c t", t=128)),
                  reads=[P6], writes=[xT])
            tg = b * NT + tt
            fw.dma("sp", xT_d, xT, lambda e, xT=xT, tg=tg: e.dma_start(out=xT_d.t[:, :, tg * 128:(tg + 1) * 128], in_=xT[:]),
                   nowaw=True, lane=tt % 2)

        def a1_proj_rope(tt):
            xT = xTt[tt % 2]
            q_ = qq[tt % 2]
            k_ = kk[tt % 2]
            PQ, PK = ((P23, P4), (P01, P7))[tt % 2]

            def proj(e, xT=xT, PQ=PQ, PK=PK):
                for (dst, c0, n) in ((PQ[:, 0:512], 0, 512), (PQ[:, 512:1024], 512, 512), (PK[:, 0:328], 1024, 328)):
                    for dc in range(8):
                        i = e.matmul(dst, lhsT=xT[:, dc, :], rhs=wtm[:, dc, c0:c0 + n], start=(dc == 0), stop=(dc == 7))
                return i
            fw.op("pe", proj, reads=[xT, wtm], writes=[PQ, PK])

        def a1_rope(tt):
            q_ = qq[tt % 2]
            k_ = kk[tt % 2]
            PQ, PK = ((P23, P4), (P01, P7))[tt % 2]
            v16 = PQ[:].rearrange("p (h d) -> p h d", d=64)
            v3 = PK[:, 0:192].rearrange("p (h d) -> p h d", d=64)
            t1, t2, t3, t4 = rope(v16, 16, tt, rt, None, PQ)
            fw.op("dve", lambda e, q_=q_: e.tensor_tensor(out=q_[:, :, 0:8], in0=t1[:], in1=t2[:], op=ALU.subtract),
                  reads=[t1, t2], writes=[q_])
            fw.op("dve", lambda e, q_=q_: e.tensor_tensor(out=q_[:, :, 8:16], in0=t3[:], in1=t4[:], op=ALU.add),
                  reads=[t3, t4], writes=[q_])
            fw.op("act", lambda e, q_=q_, v16=v16: e.activation(out=q_[:, :, 16:64], in_=v16[:, :, 16:64], func=AF.Copy),
                  reads=[PQ], writes=[q_])
            s1, s2, s3, s4 = rope(v3, 3, tt, rk, None, PK)
            fw.op("dve", lambda e: e.tensor_tensor(out=kk1[:, :, 0:8], in0=s1[:], in1=s2[:], op=ALU.subtract),
                  reads=[s1, s2], writes=[kk1])
            fw.op("dve", lambda e: e.tensor_tensor(out=kk1[:, :, 8:16], in0=s3[:], in1=s4[:], op=ALU.add),
                  reads=[s3, s4], writes=[kk1])
            fw.op("act", lambda e, v3=v3: e.activation(out=kk1[:, :, 16:64], in_=v3[:, :, 16:64], func=AF.Copy),
                  reads=[PK], writes=[kk1])
            kv5 = k_[:].rearrange("p (h a) r d -> p h a r d", a=2)
            fw.op("pool", lambda e, kv5=kv5: e.tensor_copy(out=kv5[:, :, 0, 0, :], in_=kk1[:, :, :]), reads=[kk1], writes=[k_])
            fw.op("pool", lambda e, kv5=kv5: e.tensor_copy(out=kv5[:, :, 1, 1, :], in_=kk1[:, :, :]), reads=[kk1], writes=[k_])
            fw.op("act", lambda e, tt=tt, PK=PK: e.activation(out=V_aug[:, tt, :, 0:64],
                                                             in_=PK[:, 192:320].rearrange("p (h d) -> p h d", d=64), func=AF.Copy),
                  reads=[PK], writes=[V_aug])
            fw.op("act", lambda e, tt=tt, PK=PK: e.activation(out=wi_sb[:, tt, :], in_=PK[:, 320:328], func=AF.Copy),
                  reads=[PK], writes=[wi_sb])

        def a1_tr2(tt):
            q_ = qq[tt % 2]
            k_ = kk[tt % 2]

            def tr2(e, q_=q_, k_=k_):
                q2 = q_[:].rearrange("p h d -> p (h d)")
                k2 = k_[:].rearrange("p h r d -> p (h r d)")
                for c in range(8):
                    i = e.transpose(bview(P5)[:, c, :], q2[:, c * 128:(c + 1) * 128], ident_b[:])
                for c in range(6):
                    i = e.transpose(bview(P6)[:, c, :], k2[:, c * 128:(c + 1) * 128], ident_b[:])
                return i
            fw.op("pe", tr2, reads=[q_, k_, ident_b], writes=[P5, P6])
            fw.op("act", lambda e, tt=tt: e.activation(out=featT[:, 0:8, tt * 128:(tt + 1) * 128], in_=bview(P5), func=AF.Copy),
                  reads=[P5], writes=[featT])
            fw.op("dve", lambda e, tt=tt: e.tensor_copy(out=featT[:, 8:14, tt * 128:(tt + 1) * 128], in_=bview(P6)[:, 0:6, :]),
                  reads=[P6], writes=[featT])

        a1_load(0)
        a1_load(1)
        a1_tr(0)
        for tt in range(NT):
            a1_proj_rope(tt)
            if tt + 1 < NT:
                a1_tr(tt + 1)
            a1_rope(tt)
            if tt + 2 < NT:
                a1_load(tt + 2)
            if tt >= 1:
                a1_tr2(tt - 1)
        a1_tr2(NT - 1)
        if dbg and b == 0:
            fw.dma("sp", dbg_feat, featT, lambda e: e.dma_start(out=dbg_feat.t.ap(), in_=featT[:]))
            fw.dma("sp", dbg_v, V_aug, lambda e: e.dma_start(out=dbg_v.t.ap(), in_=V_aug[:]))
            fw.dma("sp", dbg_wi, wi_sb, lambda e: e.dma_start(out=dbg_wi.t.ap(), in_=wi_sb[:]))
        if stop_after == "A1":
            break

        def gen_indexer(c, b=b):
            steps = []
            for j in range(4 * c, 4 * c + 4):
                L = 128 * (j + 1)
                for n in range((L + 511) // 512):
                    for cp in range(4):
                        steps.append((j, n, cp, min(512, L - n * 512)))
            pend = None
            acc_of = {}

            def emit_mmd(st):
                j, n, cp, N, r_, dg_ = st
                acc = acc_of[(j, n)]

                def mmd(e, acc=acc, dg_=dg_, r_=r_, cp=cp, N=N):
                    e.matmul(acc[:, 0:N], lhsT=dg_[:, 2 * cp, :], rhs=r_[:, 0, 0:N], start=(cp == 0), stop=False)
                    return e.matmul(acc[:, 0:N], lhsT=dg_[:, 2 * cp + 1, :], rhs=r_[:, 1, 0:N], start=False, stop=(cp == 3))
                fw.op("pe", mmd, reads=[dg_, r_], writes=[acc])
                if cp == 3:
                    sc_ = sc[j % 4]
                    fw.op("dve", lambda e, acc=acc, sc_=sc_, n=n, N=N: e.tensor_copy(out=sc_[:, n * 512:n * 512 + N], in_=acc[:, 0:N]),
                          reads=[acc], writes=[sc_])
                    if (n + 1) * 512 >= 128 * (j + 1):
                        fw.op("pool", lambda e, sc_=sc_, j=j: e.affine_select(
                            out=sc_[:, j * 128:(j + 1) * 128], in_=sc_[:, j * 128:(j + 1) * 128], pattern=[[-1, 128]],
                            compare_op=ALU.is_ge, fill=NEG, base=0, channel_multiplier=1), reads=[sc_], writes=[sc_])
                        if dbg and b == 0:
                            fw.dma("sp", dbg_sc, sc_, lambda e, sc_=sc_, j=j: e.dma_start(out=dbg_sc.t[j], in_=sc_[:]), nowaw=True, lane=j % 4)
            def build_dg(jb):
                if jb >= NT or (b, jb) in dg_done:
                    return
                dg_done.add((b, jb))
                dgb = dg[jb % 2]
                for h in range(8):
                    fw.op("pool", lambda e, h=h, dgb=dgb, jb=jb: e.tensor_scalar(
                        out=dgb[:, h, :], in0=ident_b[:], scalar1=wi_sb[:, jb, h:h + 1], scalar2=0.0,
                        op0=ALU.mult, op1=ALU.add), reads=[ident_b, wi_sb], writes=[dgb])
            last_j = None
            for k, (j, n, cp, N) in enumerate(steps):
                dg_ = dg[j % 2]
                new_j = (j != last_j)
                if new_j:
                    build_dg(j)
                    last_j = j
                if cp == 0:
                    acc_of[(j, n)] = (P4, P7)[nxt("acc", 2)]
                Pab = (P01, P23)[nxt("pab", 2)]
                r_ = rbuf[nxt("r", 3)]

                def mmi(e, Pab=Pab, cp=cp, j=j, n=n, N=N):
                    e.matmul(Pab[:, 0:N], lhsT=featT[:, 4 + cp, j * 128:(j + 1) * 128],
                             rhs=featT[:, 12, n * 512:n * 512 + N], start=True, stop=True)
                    return e.matmul(Pab[:, 512:512 + N], lhsT=featT[:, 4 + cp, j * 128:(j + 1) * 128],
                                    rhs=featT[:, 13, n * 512:n * 512 + N], start=True, stop=True)
                fw.op("pe", mmi, reads=[featT], writes=[Pab])
                if k % 2 == 0:
                    fw.op("act", lambda e, Pab=Pab, r_=r_, N=N: e.activation(
                        out=r_[:, :, 0:N], in_=Pab[:].rearrange("p (a n) -> p a n", a=2)[:, :, 0:N], func=AF.Relu),
                        reads=[Pab], writes=[r_])
                else:
                    fw.op("dve", lambda e, Pab=Pab, r_=r_, N=N: e.tensor_scalar(
                        out=r_[:, :, 0:N], in0=Pab[:].rearrange("p (a n) -> p a n", a=2)[:, :, 0:N], scalar1=0.0, scalar2=None,
                        op0=ALU.max), reads=[Pab], writes=[r_])
                if pend is not None:
                    emit_mmd(pend)
                if new_j:
                    build_dg(j + 1)
                pend = (j, n, cp, N, r_, dg_)
                yield
            emit_mmd(pend)
            yield

        def gen_search(c, b=b):
            mTc = mT[c % 2]
            js = list(range(4 * c, 4 * c + 4))
            Q = [jq for jq in range(4) if js[jq] >= 2]
            dveq = Q[:len(Q) // 2]
            actq = Q[len(Q) // 2:]
            for jq in range(4):
                L = 128 * (js[jq] + 1)
                val = (TOPK - 0.5) if jq in dveq else (2.0 * TOPK - 1.0 - L)
                fw.op("pool", lambda e, jq=jq, val=val: e.memset(thr4[:, jq:jq + 1], float(val)), writes=[thr4])
            fw.op("dve", lambda e: e.memset(st8[:], 0.0), writes=stT)
            for jq in range(4):
                if jq not in Q:
                    fw.op("dve", lambda e, jq=jq: e.memset(lo4[:, jq:jq + 1], -1.0e29), writes=[lo4])
            yield

            def colv(ap4, ch):
                return ap4.rearrange("p (a b) -> p a b", b=2)[:, :, ch]

            def colv3(ap8, ch):
                return ap8.rearrange("p s (a b) -> p s a b", b=2)[:, :, :, ch]
            step = SEARCH_B
            for it in range(SEARCH_IT):
                for jq in Q:
                    L = 128 * (js[jq] + 1)
                    sc_ = sc[js[jq] % 4]
                    ch = jq % 2
                    if jq in dveq:
                        fw.op("dve", lambda e, sc_=sc_, L=L, jq=jq: e.tensor_scalar(
                            out=junkD[:, 0:L], in0=sc_[:, 0:L], scalar1=st8[:, 0, jq:jq + 1], scalar2=None,
                            op0=ALU.is_ge, op1=ALU.add, accum_out=cnt4[:, jq:jq + 1]), reads=[sc_, stT[ch]], writes=[cntT[jq], junkD])
                    else:
                        fw.op("act", lambda e, sc_=sc_, L=L, jq=jq: e.activation(
                            out=junkA[:, 0:L], in_=sc_[:, 0:L], func=AF.Sign, bias=st8[:, 1, jq:jq + 1], scale=1.0,
                            accum_out=cnt4[:, jq:jq + 1]), reads=[sc_, stT[ch]], writes=[cntT[jq], junkA])
                for ch in range(2):
                    if not any(jq % 2 == ch for jq in Q):
                        continue
                    fw.op("dve", lambda e, ch=ch: e.tensor_tensor(out=colv(g4[:], ch), in0=colv(cnt4[:], ch), in1=colv(thr4[:], ch), op=ALU.is_ge),
                          reads=[cntT[ch], cntT[ch + 2], thr4], writes=[gT[ch]])
                    if it < SEARCH_IT - 1:
                        fw.op("dve", lambda e, it=it, ch=ch: e.scalar_tensor_tensor(
                            out=colv3(d8[:], ch), in0=colv(g4[:], ch).unsqueeze(1).to_broadcast([128, 2, 2]), scalar=-0.5,
                            in1=colv3(ssT[:, it, :, :], ch), op0=ALU.add, op1=ALU.mult), reads=[gT[ch], ssT], writes=[dT[ch]])
                        fw.op("dve", lambda e, ch=ch: e.tensor_tensor(out=colv3(st8[:], ch), in0=colv3(st8[:], ch), in1=colv3(d8[:], ch), op=ALU.add),
                              reads=[stT[ch], dT[ch]], writes=[stT[ch]])
                    else:
                        fw.op("dve", lambda e, step=step, ch=ch: e.tensor_scalar(out=colv(g4[:], ch), in0=colv(g4[:], ch), scalar1=-1.0, scalar2=step,
                                                                                 op0=ALU.add, op1=ALU.mult), reads=[gT[ch]], writes=[gT[ch]])
                        for jq in Q:
                            if jq % 2 == ch:
                                fw.op("dve", lambda e, jq=jq: e.tensor_tensor(out=lo4[:, jq:jq + 1], in0=g4[:, jq:jq + 1], in1=st8[:, 0, jq:jq + 1], op=ALU.add),
                                      reads=[gT[ch], stT[ch]], writes=[lo4])
                step = step * 0.5
                yield
            for jq in range(4):
                j = js[jq]
                L = 128 * (j + 1)
                sc_ = sc[j % 4]
                mk_ = mk[j % 2]
                fw.op("dve", lambda e, sc_=sc_, mk_=mk_, L=L, jq=jq: e.tensor_scalar(
                    out=mk_[:, 0:L], in0=sc_[:, 0:L], scalar1=lo4[:, jq:jq + 1], scalar2=None, op0=ALU.is_ge),
                    reads=[sc_, lo4], writes=[mk_])
                if dbg and b == 0:
                    fw.dma("sp", dbg_mk, mk_, lambda e, mk_=mk_, j=j: e.dma_start(out=dbg_mk.t[j], in_=mk_[:]), nowaw=True, lane=j % 2)
                for g0 in range(0, j + 1, 8):
                    ng = min(8, j + 1 - g0)
                    Pm = (P5, P6)[nxt("m", 2)]

                    def trm(e, Pm=Pm, mk_=mk_, g0=g0, ng=ng):
                        for i in range(ng):
                            ins = e.transpose(bview(Pm)[:, i, :], mk_[:, (g0 + i) * 128:(g0 + i + 1) * 128], ident_b[:])
                        return ins
                    fw.op("pe", trm, reads=[mk_, ident_b], writes=[Pm])
                    fw.op("act", lambda e, Pm=Pm, mTc=mTc, g0=g0, ng=ng, j=j: e.activation(
                        out=mTc[:, g0:g0 + ng, (j % 4) * 128:(j % 4 + 1) * 128], in_=bview(Pm)[:, 0:ng, :], func=AF.Identity,
                        bias=negb[:, 0:1], scale=30000.0), reads=[Pm, negb], writes=[mTc])
                yield

        def gen_attn(c, b=b, solo=False):
            mTc = mT[c % 2]
            at_ = attn_tm[0]
            n_s = 4 * c + 4
            for hp in range(4):
                kvh = hp // 2
                pend = None

                def emit_pv(st, kvh=kvh):
                    i, p_ = st

                    def mmpv(e, p_=p_, i=i, kvh=kvh, c=c):
                        ins = None
                        for hh, O in ((0, P4), (1, P7)):
                            for jj in range(4):
                                if i <= 4 * c + jj:
                                    ins = e.matmul(O[:, jj * 65:(jj + 1) * 65], lhsT=p_[:, hh, jj * 128:(jj + 1) * 128],
                                                   rhs=V_aug[:, i, kvh, :], start=(i == 0 and jj == 0),
                                                   stop=(i == 4 * c + jj), skip_group_check=True)
                        return ins
                    fw.op("pe", mmpv, reads=[p_, V_aug], writes=[P4, P7])
                for i in range(n_s):
                    off = max(0, i - 4 * c) * 128
                    Pab = (P01, P23)[nxt("pab", 2)]
                    e_ = ebuf[nxt("e", 3)]

                    def mms(e, Pab=Pab, i=i, off=off, hp=hp, kvh=kvh, c=c, mTc=mTc, solo=solo):
                        e.matmul(Pab[:, off:512], lhsT=featT[:, 8 + 2 * kvh, i * 128:(i + 1) * 128],
                                 rhs=featT[:, hp, c * 512 + off:(c + 1) * 512], start=True, stop=solo)
                        ins = e.matmul(Pab[:, 512 + off:1024], lhsT=featT[:, 9 + 2 * kvh, i * 128:(i + 1) * 128],
                                       rhs=featT[:, hp, c * 512 + off:(c + 1) * 512], start=True, stop=solo)
                        if solo:
                            return ins
                        e.matmul(Pab[:, off:512], lhsT=ident_b[:], rhs=mTc[:, i, off:512], start=False, stop=True)
                        return e.matmul(Pab[:, 512 + off:1024], lhsT=ident_b[:], rhs=mTc[:, i, off:512], start=False, stop=True)
                    fw.op("pe", mms, reads=[featT, mTc, ident_b], writes=[Pab])
                    if solo:
                        N_ = 512 - off
                        fw.op("dve", lambda e, Pab=Pab, i=i, off=off, N_=N_, mTc=mTc: e.tensor_tensor(
                            out=Pab[:].rearrange("p (a n) -> p a n", a=2)[:, :, off:512],
                            in0=Pab[:].rearrange("p (a n) -> p a n", a=2)[:, :, off:512],
                            in1=mTc[:, i, off:512].unsqueeze(1).to_broadcast([128, 2, N_]), op=ALU.add),
                            reads=[Pab, mTc], writes=[Pab])
                    fw.op("act", lambda e, Pab=Pab, e_=e_, off=off: e.activation(
                        out=e_[:, :, off:512], in_=Pab[:].rearrange("p (a n) -> p a n", a=2)[:, :, off:512],
                        func=AF.Exp, scale=0.125), reads=[Pab], writes=[e_])
                    if pend is not None:
                        emit_pv(pend)
                    pend = (i, e_)
                    yield
                emit_pv(pend)
                for hh, O in ((0, P4), (1, P7)):
                    hd = 2 * hp + hh
                    O3 = O[:, 0:260].rearrange("p (j d) -> p j d", d=65)
                    fw.op("dve", lambda e, O3=O3: e.reciprocal(out=rec[:], in_=O3[:, :, 64]), reads=[O], writes=[rec])
                    fw.op("dve", lambda e, O3=O3, hd=hd, at_=at_: e.tensor_tensor(
                        out=at_[:, :, hd * 64:(hd + 1) * 64], in0=O3[:, :, 0:64],
                        in1=rec[:].unsqueeze(2).to_broadcast([128, 4, 64]), op=ALU.mult), reads=[O, rec], writes=[at_])
                yield
            aT_ = attnT_c[c % 2]
            for jj in range(4):
                Pm = (P5, P6)[nxt("m", 2)]

                def tra(e, Pm=Pm, jj=jj, at_=at_):
                    for hc in range(4):
                        ins = e.transpose(bview(Pm)[:, hc, :], at_[:, jj, hc * 128:(hc + 1) * 128], ident_b[:])
                    return ins
                fw.op("pe", tra, reads=[at_, ident_b], writes=[Pm])
                fw.op("act", lambda e, Pm=Pm, jj=jj, aT_=aT_: e.activation(
                    out=aT_[:, :, jj * 128:(jj + 1) * 128], in_=bview(Pm)[:, 0:4, :], func=AF.Copy), reads=[Pm], writes=[aT_])
            g = b * 4 + c
            fw.dma("sp", attnT_d, aT_, lambda e, g=g, aT_=aT_: e.dma_start(out=attnT_d.t[:, :, g * 512:(g + 1) * 512], in_=aT_[:]),
                   nowaw=True, lane=c % 2)
            yield

        def run(gen):
            for _ in gen:
                pass

        def interleave(ga, na, gb, nb):
            da = db = False
            ia = ib = 0
            while not (da and db):
                ta = (ia + 1) * nb
                tb = (ib + 1) * na
                if not da and (db or ta <= tb):
                    try:
                        next(ga)
                        ia += 1
                    except StopIteration:
                        da = True
                else:
                    try:
                        next(gb)
                        ib += 1
                    except StopIteration:
                        db = True

        def precast(ex):
            for src, dst in ((wg_d, wgc_d), (wu_d, wuc_d), (wd_d, wdc_d)):
                fw.dma("pool", dst, src, lambda e, src=src, dst=dst, ex=ex: e.dma_start(out=dst.t[ex], in_=src.t[ex]),
                       nowaw=True, lane="c")
        PC = (2, 4, 5, 5)
        pc0 = 16 * b

        def precast_chunk(c, b=b):
            z0 = (b * 4 + c) * 12
            for zi in range(z0, z0 + 12):
                fw.dma("pool", xs_d, zeros_d, lambda e, zi=zi: e.dma_start(out=xs_d.t[zi * 128:(zi + 1) * 128, :], in_=zeros_d.t.ap()),
                       nowaw=True, lane="z")
            if stop_after is None:
                for ex in range(pc0 + sum(PC[:c]), pc0 + sum(PC[:c + 1])):
                    precast(ex)

        precast_chunk(0)
        run(gen_indexer(0))
        run(gen_search(0))
        for c in range(1, 4):
            precast_chunk(c)
            run(gen_indexer(c))
            if stop_after == "A2":
                run(gen_search(c))
            else:
                interleave(gen_search(c), SEARCH_IT + 6, gen_attn(c - 1), 16 * c + 6)
        if stop_after != "A2":
            run(gen_attn(3, solo=False))

    outs = [out_d]
    if stop_after in ("A1", "A2", "A3"):
        tail = [attnT_d]
        if dbg:
            tail += [dbg_feat, dbg_v, dbg_wi, dbg_sc, dbg_mk]
        fw.wait_all("sp", [t for t in tail if t.w])
        fw.finish()
        return nc

    fw.barrier()
    fw.release(base_mark)

    wfm = fw.sb("wfm", [128, 8, 3584], BF16)
    for half in range(2):
        for ch in range(2):
            fw.dma("pool", wfm, wfm_d, lambda e, half=half, ch=ch: e.dma_start(
                out=wfm[:, half * 4:(half + 1) * 4, ch * 1792:(ch + 1) * 1792],
                in_=wfm_d.t[half * 512:(half + 1) * 512, ch * 1792:(ch + 1) * 1792].rearrange("(c p) n -> p c n", p=128)),
                nowaw=(half + ch > 0), lane=half * 2 + ch)
    waup = fw.sb("waup", [128, 4, D], BF16)
    wcup = fw.sb("wcup", [128, 4, D], BF16)
    wout = fw.sb("wout", [128, 8, D], BF16)
    fw.dma("pool", waup, waup_d, lambda e: e.dma_start(out=waup[:], in_=waup_d.t.ap().rearrange("(c p) n -> p c n", p=128)))
    fw.dma("pool", wcup, wcup_d, lambda e: e.dma_start(out=wcup[:], in_=wcup_d.t.ap().rearrange("(c p) n -> p c n", p=128)))
    fw.dma("pool", wout, wout_d, lambda e: e.dma_start(out=wout[:], in_=wout_d.t.ap().rearrange("(c p) n -> p c n", p=128)))
    gb = fw.sb("gb", [128, 16], F32)
    fw.dma("sp", gb, gb_d, lambda e: e.dma_start(out=gb[:], in_=gb_d.t.ap()))
    cw = fw.sb("cw", [128, 4, 3], F32)
    fw.dma("sp", cw, convw_d, lambda e: e.dma_start(out=cw[:], in_=convw_d.t.ap()))
    ln1g = fw.sb("ln1g", [128, D], F32)
    ln1b = fw.sb("ln1b", [128, D], F32)
    fw.dma("sp", ln1g, lnp_d, lambda e: e.dma_start(out=ln1g[:], in_=lnp_d.t[0:1, :].partition_broadcast(128)))
    fw.dma("sp", ln1b, lnp_d, lambda e: e.dma_start(out=ln1b[:], in_=lnp_d.t[1:2, :].partition_broadcast(128)))
    wr = fw.sb("wr", [128, 8, 36], F32)
    fw.dma("sp", wr, wr_d, lambda e: e.dma_start(out=wr[:], in_=wr_d.t.ap().rearrange("(c p) n -> p c n", p=128)))
    rb = fw.sb("rb", [128, 36], F32)
    fw.dma("sp", rb, rb_d, lambda e: e.dma_start(out=rb[:], in_=rb_d.t[0:1, :].partition_broadcast(128)))
    xs4 = fw.sb("xs4", [128, 4, D], F32)
    xs4t = [T(xs4.t, f"xs4_{i}") for i in range(4)]
    xTc = fw.sb("xTc", [128, 8, 512], BF16)
    aTc = [fw.sb(f"aTc{i}", [128, 4, 512], BF16) for i in range(1)]
    cin2 = [fw.sb(f"cin{i}", [128, 512], F32) for i in range(2)]
    u1 = fw.sb("u1", [128, 4, 514], F32)
    ycv2 = [fw.sb(f"ycv{i}", [128, 512], F32) for i in range(2)]
    convT = fw.sb("convT", [128, 4, 512], BF16)
    gA2 = [fw.sb(f"gA{i}", [128, 512], F32) for i in range(2)]
    gC2 = [fw.sb(f"gC{i}", [128, 512], F32) for i in range(2)]
    m12 = [fw.sb(f"m1{i}", [128, 512], F32) for i in range(2)]
    m22 = [fw.sb(f"m2{i}", [128, 512], F32) for i in range(2)]
    mergedT = fw.sb("mergedT", [128, 8, 512], BF16)
    ys = [fw.sb(f"ys{i}", [128, D], F32) for i in range(4)]
    hbf = [fw.sb(f"hbf{i}", [128, D], BF16) for i in range(4)]
    hT2 = [fw.sb(f"hT{i}", [128, 8, 128], F32) for i in range(1)]
    st64 = fw.sb("st64", [128, 4, 2, 6], F32)
    mv4 = fw.sb("mv4", [128, 4, 2], F32)
    sd4 = fw.sb("sd4", [128, 4], F32)
    rstd4 = fw.sb("rstd4", [128, 4], F32)
    nmr4 = fw.sb("nmr4", [128, 4], F32)
    epsb = fw.sb("epsb", [128, 1], F32)
    fw.op("dve", lambda e: e.memset(epsb[:], LN_EPS), writes=[epsb])
    lg4 = fw.sb("lg4", [128, 4, 36], F32)
    gmx4 = fw.sb("gmx4", [128, 4], F32)
    ohg4 = fw.sb("ohg4", [128, 4, 4], F32)
    sh4 = fw.sb("sh4", [128, 4, 4], F32)
    seg4 = fw.sb("seg4", [128, 4], F32)
    pgrp4 = fw.sb("pgrp4", [128, 4], F32)
    prod4 = fw.sb("prod4", [128, 4, 4, 8], F32)
    esel4 = fw.sb("esel4", [128, 4, 8], F32)
    mx14 = fw.sb("mx14", [128, 4], F32)
    mx24 = fw.sb("mx24", [128, 4], F32)
    oh14 = fw.sb("oh14", [128, 4, 8], F32)
    oh24 = fw.sb("oh24", [128, 4, 8], F32)
    e24 = fw.sb("e24", [128, 4, 8], F32)
    ed4 = fw.sb("ed4", [128, 4], F32)
    w14 = fw.sb("w14", [128, 4], F32)
    E14 = fw.sb("E14", [128, 4, 4, 8], F32)
    E24 = fw.sb("E24", [128, 4, 4, 8], F32)
    Mm4 = fw.sb("Mm4", [128, 4, 32], F32)
    Macc = fw.sb("Macc", [128, 32], F32)
    msum = fw.sb("msum", [128, 32], F32)
    RC4 = fw.sb("RC4", [128, 4, 32], F32)
    tmp4 = fw.sb("tmp4", [128, 4, 32], F32)
    dstf4 = fw.sb("dstf4", [128, 4, 2], F32)
    fw.op("dve", lambda e: e.memset(Macc[:], 0.0), writes=[Macc])

    def gen_front(g):
            b, c = g // 4, g % 4
            aT_ = aTc[0]
            fw.dma("sp", xTc, xT_d, lambda e, g=g: e.dma_start(out=xTc[:], in_=xT_d.t[:, :, g * 512:(g + 1) * 512]))
            fw.dma("sp", aT_, attnT_d, lambda e, aT_=aT_, g=g: e.dma_start(out=aT_[:], in_=attnT_d.t[:, :, g * 512:(g + 1) * 512]))
            for jj in range(4):
                fw.dma("sp", xs4t[jj], x_d, lambda e, b=b, c=c, jj=jj: e.dma_start(
                    out=xs4[:, jj, :], in_=x_d.t[b, c * 512 + jj * 128:c * 512 + (jj + 1) * 128, :]))
            yield
            uc = u1
            if c == 0:
                fw.op("pool", lambda e, uc=uc: e.memset(uc[:, :, 0:2], 0.0), writes=[uc])
            for cc in range(4):
                QQ = (Q0, Q1, Q2, Q3)
                TIn, TC, TBb = QQ[(3 * cc) % 4], QQ[(3 * cc + 1) % 4], QQ[(3 * cc + 2) % 4]
                in_ap, c_ap, b_ap = QAP[TIn], QAP[TC], QAP[TBb]
                cin, ycv = cin2[cc % 2], ycv2[cc % 2]

                for (Tb, dst, col0) in ((TIn, in_ap, cc * 128), (TC, c_ap, 1024 + cc * 128), (TBb, b_ap, 512 + cc * 128)):
                    def mmc(e, dst=dst, col0=col0):
                        for dc in range(8):
                            ins = e.matmul(dst, lhsT=wfm[:, dc, col0:col0 + 128], rhs=xTc[:, dc, :], start=(dc == 0), stop=(dc == 7))
                        return ins
                    fw.op("pe", mmc, reads=[wfm, xTc], writes=[Tb])
                fw.op("act", lambda e, cin=cin, in_ap=in_ap: e.activation(out=cin[:], in_=in_ap, func=AF.Copy), reads=[TIn], writes=[cin])
                fw.op("dve", lambda e, cc=cc, uc=uc, cin=cin, c_ap=c_ap: e.tensor_tensor(out=uc[:, cc, 2:514], in0=c_ap, in1=cin[:], op=ALU.mult),
                      reads=[TC, cin], writes=[uc])
                fw.op("act", lambda e, cc=cc, uc=uc, ycv=ycv: e.activation(out=ycv[:], in_=uc[:, cc, 2:514], func=AF.Identity, scale=cw[:, cc, 2:3]),
                      reads=[uc, cw], writes=[ycv])
                fw.op("dve", lambda e, cc=cc, uc=uc, ycv=ycv: e.scalar_tensor_tensor(out=ycv[:], in0=uc[:, cc, 1:513], scalar=cw[:, cc, 1:2], in1=ycv[:],
                                                                                     op0=ALU.mult, op1=ALU.add), reads=[uc, cw, ycv], writes=[ycv])
                fw.op("dve", lambda e, cc=cc, uc=uc, ycv=ycv: e.scalar_tensor_tensor(out=ycv[:], in0=uc[:, cc, 0:512], scalar=cw[:, cc, 0:1], in1=ycv[:],
                                                                                     op0=ALU.mult, op1=ALU.add), reads=[uc, cw, ycv], writes=[ycv])
                fw.op("dve", lambda e, cc=cc, ycv=ycv, b_ap=b_ap: e.tensor_tensor(out=convT[:, cc, :], in0=b_ap, in1=ycv[:], op=ALU.mult),
                      reads=[TBb, ycv], writes=[convT])
                yield
            yield
            if c < 3:
                fw.op("pool", lambda e, uc=uc: e.tensor_copy(out=uc[:, :, 0:2], in_=uc[:, :, 512:514]), reads=[uc], writes=[uc])
            yield
            for dmc in range(8):
                TGA, TGC, TA, TB = Q0, Q1, Q2, Q3
                A_ap, B_ap, GA_ap, GC_ap = QAP[TA], QAP[TB], QAP[TGA], QAP[TGC]
                gA, gC, m1, m2 = gA2[dmc % 2], gC2[dmc % 2], m12[dmc % 2], m22[dmc % 2]

                for (Tb, dst, col0) in ((TGA, GA_ap, 1536 + dmc * 128), (TGC, GC_ap, 2560 + dmc * 128)):
                    def mmg(e, dst=dst, col0=col0):
                        for dc in range(8):
                            ins = e.matmul(dst, lhsT=wfm[:, dc, col0:col0 + 128], rhs=xTc[:, dc, :], start=(dc == 0), stop=(dc == 7))
                        return ins
                    fw.op("pe", mmg, reads=[wfm, xTc], writes=[Tb])
                fw.op("act", lambda e, dmc=dmc, gA=gA, GA_ap=GA_ap: e.activation(out=gA[:], in_=GA_ap, func=AF.Sigmoid, bias=gb[:, dmc:dmc + 1], scale=1.0),
                      reads=[TGA, gb], writes=[gA])
                fw.op("act", lambda e, dmc=dmc, gC=gC, GC_ap=GC_ap: e.activation(out=gC[:], in_=GC_ap, func=AF.Sigmoid, bias=gb[:, 8 + dmc:9 + dmc], scale=1.0),
                      reads=[TGC, gb], writes=[gC])

                def mmua(e, dmc=dmc, aT_=aT_, A_ap=A_ap):
                    for hc in range(4):
                        ins = e.matmul(A_ap, lhsT=waup[:, hc, dmc * 128:(dmc + 1) * 128], rhs=aT_[:, hc, :], start=(hc == 0), stop=(hc == 3))
                    return ins
                fw.op("pe", mmua, reads=[waup, aT_], writes=[TA])

                def mmub(e, dmc=dmc, B_ap=B_ap):
                    for cc in range(4):
                        ins = e.matmul(B_ap, lhsT=wcup[:, cc, dmc * 128:(dmc + 1) * 128], rhs=convT[:, cc, :], start=(cc == 0), stop=(cc == 3))
                    return ins
                fw.op("pe", mmub, reads=[wcup, convT], writes=[TB])
                fw.op("dve", lambda e, m1=m1, gA=gA, A_ap=A_ap: e.tensor_tensor(out=m1[:], in0=A_ap, in1=gA[:], op=ALU.mult), reads=[TA, gA], writes=[m1])
                fw.op("dve", lambda e, m2=m2, gC=gC, B_ap=B_ap: e.tensor_tensor(out=m2[:], in0=B_ap, in1=gC[:], op=ALU.mult), reads=[TB, gC], writes=[m2])
                fw.op("pool", lambda e, dmc=dmc, m1=m1, m2=m2: e.tensor_tensor(out=mergedT[:, dmc, :], in0=m1[:], in1=m2[:], op=ALU.add),
                      reads=[m1, m2], writes=[mergedT])
                yield
            yield

    def gen_back(g):
            AXX = mybir.AxisListType.X
            mixP = [([Q0, Q1], P01[:, 0:512], P01[:, 512:1024]), ([Q2, Q3], P23[:, 0:512], P23[:, 512:1024]),
                    ([P4, P5], P4[:, 0:512], P5[:, 0:512]), ([P6, P7], P6[:, 0:512], P7[:, 0:512])]
            yield
            for jj in range(4):
                Ts, lo_ap, hi_ap = mixP[jj]

                def mmo(e, jj=jj, lo_ap=lo_ap, hi_ap=hi_ap):
                    for dst, hf in ((lo_ap, 0), (hi_ap, 1)):
                        for dmc in range(8):
                            ins = e.matmul(dst, lhsT=mergedT[:, dmc, jj * 128:(jj + 1) * 128],
                                           rhs=wout[:, dmc, hf * 512:(hf + 1) * 512], start=(dmc == 0), stop=(dmc == 7))
                    return ins
                fw.op("pe", mmo, reads=[mergedT, wout], writes=Ts)
            yield
            for jj in range(4):
                Ts, lo_ap, hi_ap = mixP[jj]
                y_ = ys[jj]
                for hf, ap in ((0, lo_ap), (1, hi_ap)):
                    fw.op("dve", lambda e, y_=y_, jj=jj, hf=hf, ap=ap: e.scalar_tensor_tensor(
                        out=y_[:, hf * 512:(hf + 1) * 512], in0=xs4[:, jj, hf * 512:(hf + 1) * 512], scalar=ALPHA, in1=ap,
                        op0=ALU.mult, op1=ALU.add), reads=[xs4t[jj]] + Ts, writes=[y_])
            yield
            for jj in range(4):
                y_ = ys[jj]
                for hf in range(2):
                    fw.op("dve", lambda e, hf=hf, y_=y_, jj=jj: e.bn_stats(out=st64[:, jj, hf, :], in_=y_[:, hf * 512:(hf + 1) * 512]),
                          reads=[y_], writes=[st64])
            yield
            for jj in range(4):
                fw.op("dve", lambda e, jj=jj: e.bn_aggr(out=mv4[:, jj, :], in_=st64[:, jj, :, :].rearrange("p a s -> p (a s)")),
                      reads=[st64], writes=[mv4])
            fw.op("act", lambda e: e.activation(out=sd4[:], in_=mv4[:, :, 1], func=AF.Sqrt, bias=epsb[:, 0:1], scale=1.0),
                  reads=[mv4, epsb], writes=[sd4])
            fw.op("dve", lambda e: e.reciprocal(out=rstd4[:], in_=sd4[:]), reads=[sd4], writes=[rstd4])
            fw.op("dve", lambda e: e.scalar_tensor_tensor(out=nmr4[:], in0=mv4[:, :, 0], scalar=-1.0, in1=rstd4[:], op0=ALU.mult, op1=ALU.mult),
                  reads=[mv4, rstd4], writes=[nmr4])
            yield
            for jj in range(4):
                y_ = ys[jj]
                fw.op("act", lambda e, y_=y_, jj=jj: e.activation(out=y_[:], in_=y_[:], func=AF.Identity, bias=nmr4[:, jj:jj + 1], scale=rstd4[:, jj:jj + 1]),
                      reads=[y_, nmr4, rstd4], writes=[y_])
            yield
            for jj in range(4):
                y_ = ys[jj]
                fw.op("dve", lambda e, y_=y_: e.tensor_tensor(out=y_[:], in0=y_[:], in1=ln1g[:], op=ALU.mult), reads=[y_, ln1g], writes=[y_])
                fw.op("pool", lambda e, y_=y_: e.tensor_tensor(out=y_[:], in0=y_[:], in1=ln1b[:], op=ALU.add), reads=[y_, ln1b], writes=[y_])
            yield
            for jj in range(4):
                y_ = ys[jj]
                ti = g * 4 + jj
                fw.dma("sp", h_d, y_, lambda e, y_=y_, ti=ti: e.dma_start(out=h_d.t[ti * 128:(ti + 1) * 128, :], in_=y_[:]), nowaw=True, lane=jj)
                fw.op("act", lambda e, y_=y_, jj=jj: e.activation(out=hbf[jj][:], in_=y_[:], func=AF.Copy), reads=[y_], writes=[hbf[jj]])
            yield
            for jj in range(4):
                y_ = ys[jj]
                hT_ = hT2[0]

                def trh(e, y_=y_):
                    for dc in range(8):
                        Pq = P4 if dc < 4 else P5
                        ins = e.transpose(Pq[:, (dc % 4) * 128:(dc % 4 + 1) * 128], y_[:, dc * 128:(dc + 1) * 128], IDF())
                    return ins
                fw.op("pe", trh, reads=[y_, consts], writes=[P4, P5])
                fw.op("act", lambda e, hT_=hT_: e.activation(out=hT_[:, 0:4, :], in_=P4[:].rearrange("p (c t) -> p c t", t=128), func=AF.Copy),
                      reads=[P4], writes=[hT_])
                fw.op("act", lambda e, hT_=hT_: e.activation(out=hT_[:, 4:8, :], in_=P5[:].rearrange("p (c t) -> p c t", t=128), func=AF.Copy),
                      reads=[P5], writes=[hT_])

                def mmr(e, hT_=hT_, jj=jj):
                    for dc in range(8):
                        ins = e.matmul(P6[:, jj * 64:jj * 64 + 36], lhsT=hT_[:, dc, :], rhs=wr[:, dc, :], start=(dc == 0), stop=(dc == 7))
                    return ins
                fw.op("pe", mmr, reads=[hT_, wr], writes=[P6])
            yield
            V = "dve"
            lgG = lg4[:, :, 0:4]
            fw.op(V, lambda e: e.tensor_tensor(out=lg4[:], in0=P6[:, 0:256].rearrange("p (t n) -> p t n", n=64)[:, :, 0:36],
                                               in1=rb[:].unsqueeze(1).to_broadcast([128, 4, 36]), op=ALU.add), reads=[P6, rb], writes=[lg4])
            fw.op(V, lambda e: e.tensor_reduce(out=gmx4[:], in_=lgG, axis=AXX, op=ALU.max), reads=[lg4], writes=[gmx4])
            fw.op(V, lambda e: e.tensor_tensor(out=ohg4[:], in0=lgG, in1=gmx4[:].unsqueeze(2).to_broadcast([128, 4, 4]), op=ALU.is_ge),
                  reads=[lg4, gmx4], writes=[ohg4])
            fw.op(V, lambda e: e.tensor_tensor(out=sh4[:], in0=lgG, in1=gmx4[:].unsqueeze(2).to_broadcast([128, 4, 4]), op=ALU.subtract),
                  reads=[lg4, gmx4], writes=[sh4])
            fw.op("act", lambda e: e.activation(out=sh4[:], in_=sh4[:], func=AF.Exp), reads=[sh4], writes=[sh4])
            fw.op(V, lambda e: e.tensor_reduce(out=seg4[:], in_=sh4[:], axis=AXX, op=ALU.add), reads=[sh4], writes=[seg4])
            fw.op(V, lambda e: e.reciprocal(out=pgrp4[:], in_=seg4[:]), reads=[seg4], writes=[pgrp4])
            fw.op(V, lambda e: e.tensor_tensor(out=prod4[:], in0=lg4[:, :, 4:36].rearrange("p t (g j) -> p t g j", j=8),
                                               in1=ohg4[:].unsqueeze(3).to_broadcast([128, 4, 4, 8]), op=ALU.mult), reads=[lg4, ohg4], writes=[prod4])
            fw.op(V, lambda e: e.tensor_reduce(out=esel4[:], in_=prod4[:].rearrange("p t g j -> p t j g"), axis=AXX, op=ALU.add),
                  reads=[prod4], writes=[esel4])
            fw.op(V, lambda e: e.tensor_reduce(out=mx14[:], in_=esel4[:], axis=AXX, op=ALU.max), reads=[esel4], writes=[mx14])
            fw.op(V, lambda e: e.tensor_tensor(out=oh14[:], in0=esel4[:], in1=mx14[:].unsqueeze(2).to_broadcast([128, 4, 8]), op=ALU.is_ge),
                  reads=[esel4, mx14], writes=[oh14])
            fw.op(V, lambda e: e.scalar_tensor_tensor(out=e24[:].rearrange("p t j -> p (t j)"), in0=oh14[:].rearrange("p t j -> p (t j)"), scalar=NEG,
                                                      in1=esel4[:].rearrange("p t j -> p (t j)"), op0=ALU.mult, op1=ALU.add),
                  reads=[oh14, esel4], writes=[e24])
            fw.op(V, lambda e: e.tensor_reduce(out=mx24[:], in_=e24[:], axis=AXX, op=ALU.max), reads=[e24], writes=[mx24])
            fw.op(V, lambda e: e.tensor_tensor(out=oh24[:], in0=e24[:], in1=mx24[:].unsqueeze(2).to_broadcast([128, 4, 8]), op=ALU.is_ge),
                  reads=[e24, mx24], writes=[oh24])
            fw.op(V, lambda e: e.tensor_tensor(out=ed4[:], in0=mx24[:], in1=mx14[:], op=ALU.subtract), reads=[mx14, mx24], writes=[ed4])
            fw.op("act", lambda e: e.activation(out=ed4[:], in_=ed4[:], func=AF.Exp), reads=[ed4], writes=[ed4])
            fw.op(V, lambda e: e.tensor_scalar(out=w14[:], in0=ed4[:], scalar1=1.0, scalar2=None, op0=ALU.add), reads=[ed4], writes=[w14])
            fw.op(V, lambda e: e.reciprocal(out=w14[:], in_=w14[:]), reads=[w14], writes=[w14])
            fw.op(V, lambda e, g=g: e.tensor_tensor(out=gate_all[:, g * 4:(g + 1) * 4, 0], in0=w14[:], in1=pgrp4[:], op=ALU.mult),
                  reads=[w14, pgrp4], writes=[gate_all])
            fw.op(V, lambda e, g=g: e.tensor_tensor(out=gate_all[:, g * 4:(g + 1) * 4, 1], in0=gate_all[:, g * 4:(g + 1) * 4, 0], in1=ed4[:], op=ALU.mult),
                  reads=[ed4, gate_all], writes=[gate_all])
            for Ek, ohk in ((E14, oh14), (E24, oh24)):
                fw.op(V, lambda e, Ek=Ek, ohk=ohk: e.tensor_tensor(out=Ek[:], in0=ohg4[:].unsqueeze(3).to_broadcast([128, 4, 4, 8]),
                                                                  in1=ohk[:].unsqueeze(2).to_broadcast([128, 4, 4, 8]), op=ALU.mult),
                      reads=[ohg4, ohk], writes=[Ek])
            fw.op(V, lambda e: e.tensor_tensor(out=Mm4[:], in0=E14[:].rearrange("p t g j -> p t (g j)"), in1=E24[:].rearrange("p t g j -> p t (g j)"), op=ALU.add),
                  reads=[E14, E24], writes=[Mm4])

            yield
            def mmrank(e):
                for jj in range(4):
                    dst = P7[:, jj * 32:(jj + 1) * 32]
                    e.matmul(dst, lhsT=TRI(), rhs=Mm4[:, jj, :], start=True, stop=False)
                    for i in range(jj):
                        e.matmul(dst, lhsT=ones_f[:], rhs=Mm4[:, i, :], start=False, stop=False)
                    ins = e.matmul(dst, lhsT=ones_f[:], rhs=Macc[:], start=False, stop=True)
                return ins
            fw.op("pe", mmrank, reads=[consts, ones_f, Mm4, Macc], writes=[P7])
            fw.op(V, lambda e: e.tensor_scalar(out=RC4[:], in0=P7[:, 0:128].rearrange("p (t n) -> p t n", n=32), scalar1=float(CAP - 1), scalar2=None, op0=ALU.min),
                  reads=[P7], writes=[RC4])
            fw.op(V, lambda e: e.tensor_tensor(out=RC4[:], in0=RC4[:], in1=ECAP().unsqueeze(1).to_broadcast([128, 4, 32]), op=ALU.add),
                  reads=[RC4, consts], writes=[RC4])
            fw.op(V, lambda e: e.tensor_reduce(out=msum[:], in_=Mm4[:].rearrange("p t n -> p n t"), axis=AXX, op=ALU.add), reads=[Mm4], writes=[msum])
            fw.op(V, lambda e: e.tensor_tensor(out=Macc[:], in0=Macc[:], in1=msum[:], op=ALU.add), reads=[Macc, msum], writes=[Macc])
            yield
            for k_i, Ek in ((0, E14), (1, E24)):
                fw.op(V, lambda e, Ek=Ek: e.tensor_tensor(out=tmp4[:], in0=Ek[:].rearrange("p t g j -> p t (g j)"), in1=RC4[:], op=ALU.mult),
                      reads=[Ek, RC4], writes=[tmp4])
                fw.op(V, lambda e, k_i=k_i: e.tensor_reduce(out=dstf4[:, :, k_i], in_=tmp4[:], axis=AXX, op=ALU.add), reads=[tmp4], writes=[dstf4])
            fw.op(V, lambda e, g=g: e.tensor_copy(out=dest_all[:, g * 4:(g + 1) * 4, :], in_=dstf4[:]), reads=[dstf4], writes=[dest_all])
            yield
            for jj in range(4):
                ti = g * 4 + jj
                for k_i in range(2):
                    fw.dma("pool", xs_d, hbf[jj], lambda e, jj=jj, ti=ti, k_i=k_i: e.indirect_dma_start(
                        out=xs_d.t[:, :], out_offset=bass.IndirectOffsetOnAxis(ap=dest_all[:, ti, k_i:k_i + 1], axis=0),
                        in_=hbf[jj][:, :], in_offset=None), extra_reads=[dest_all], nowaw=(ti > 0 or k_i > 0), lane="s%d" % (jj * 2 + k_i))


            yield

    run(gen_front(0))
    for g in range(8):
        gbk = gen_back(g)
        for _ in range(3):
            next(gbk)
        if g + 1 < 8:
            interleave(gbk, 11, gen_front(g + 1), 18)
        else:
            run(gbk)

    if stop_after == "B":
        fw.wait_all("sp", [h_d, xs_d])
        fw.finish()
        return nc

    fw.barrier()
    fw.release(base_mark)

    wgb = [fw.sb(f"wgb{i}", [128, 8, 512], BF16) for i in range(3)]
    wub = [fw.sb(f"wub{i}", [128, 8, 512], BF16) for i in range(3)]
    wdb = [fw.sb(f"wdb{i}", [128, 4, D], BF16) for i in range(4)]
    xsb = [fw.sb(f"xsb{i}", [128, 3, D], BF16) for i in range(3)]
    xsT = [fw.sb(f"xsT{i}", [128, 8, CAP], BF16) for i in range(2)]
    sg = [fw.sb(f"sg{i}", [128, CAP], F32) for i in range(2)]
    hidT = [fw.sb(f"hidT{i}", [128, 4, CAP], BF16) for i in range(2)]
    ysb = [fw.sb(f"ysb{i}", [128, D], F32) for i in range(3)]
    ycn = [0]

    def moe_load(ex):
        wg_, wu_, wd_, xs_ = wgb[ex % 3], wub[ex % 3], wdb[ex % 4], xsb[ex % 3]
        fw.dma("sp", xs_, xs_d, lambda e, xs_=xs_, ex=ex: e.dma_start(
            out=xs_[:], in_=xs_d.t[ex * CAP:(ex + 1) * CAP, :].rearrange("(s p) d -> p s d", p=128)))
        fw.dma("sp", wg_, wgc_d, lambda e, wg_=wg_, ex=ex: e.dma_start(out=wg_[:], in_=wgc_d.t[ex].rearrange("(c p) n -> p c n", p=128)))
        fw.dma("sp", wu_, wuc_d, lambda e, wu_=wu_, ex=ex: e.dma_start(out=wu_[:], in_=wuc_d.t[ex].rearrange("(c p) n -> p c n", p=128)))
        fw.dma("sp", wd_, wdc_d, lambda e, wd_=wd_, ex=ex: e.dma_start(out=wd_[:], in_=wdc_d.t[ex].rearrange("(c p) n -> p c n", p=128)))

    def moe_T(ex):
        xs_, xT_ = xsb[ex % 3], xsT[ex % 2]
        for st in range(3):
            yield
            Pm = (P5, P6)[nxt("m", 2)]

            def trs(e, Pm=Pm, xs_=xs_, st=st):
                for dc in range(8):
                    ins = e.transpose(bview(Pm)[:, dc, :], xs_[:, st, dc * 128:(dc + 1) * 128], ident_b[:])
                return ins
            fw.op("pe", trs, reads=[xs_, ident_b], writes=[Pm])
            if st == 1:
                fw.op("dve", lambda e, Pm=Pm, xT_=xT_, st=st: e.tensor_copy(out=xT_[:, :, st * 128:(st + 1) * 128], in_=bview(Pm)),
                      reads=[Pm], writes=[xT_])
            else:
                fw.op("act", lambda e, Pm=Pm, xT_=xT_, st=st: e.activation(out=xT_[:, :, st * 128:(st + 1) * 128], in_=bview(Pm), func=AF.Copy),
                      reads=[Pm], writes=[xT_])

    def moe_GU(ex):
        wg_, wu_ = wgb[ex % 3], wub[ex % 3]
        xT_, hid_ = xsT[ex % 2], hidT[ex % 2]
        for fc in range(4):
            yield
            Pgu = (P01, P23)[nxt("pab", 2)]
            sg_ = sg[fc % 2]

            def mmgu(e, Pgu=Pgu, fc=fc, wg_=wg_, wu_=wu_, xT_=xT_):
                for dc in range(8):
                    e.matmul(Pgu[:, 0:CAP], lhsT=wg_[:, dc, fc * 128:(fc + 1) * 128], rhs=xT_[:, dc, :], start=(dc == 0), stop=(dc == 7))
                for dc in range(8):
                    ins = e.matmul(Pgu[:, 512:512 + CAP], lhsT=wu_[:, dc, fc * 128:(fc + 1) * 128], rhs=xT_[:, dc, :], start=(dc == 0), stop=(dc == 7))
                return ins
            fw.op("pe", mmgu, reads=[wg_, wu_, xT_], writes=[Pgu])
            fw.op("act", lambda e, Pgu=Pgu, sg_=sg_: e.activation(out=sg_[:], in_=Pgu[:, 0:CAP], func=AF.Silu), reads=[Pgu], writes=[sg_])
            fw.op("dve", lambda e, Pgu=Pgu, sg_=sg_, hid_=hid_, fc=fc: e.tensor_tensor(out=hid_[:, fc, :], in0=Pgu[:, 512:512 + CAP], in1=sg_[:], op=ALU.mult),
                  reads=[Pgu, sg_], writes=[hid_])

    def moe_DN(ex):
        wd_, hid_ = wdb[ex % 4], hidT[ex % 2]
        for st in range(3):
            yield
            yc = ycn[0]
            ycn[0] += 1
            y_ = ysb[yc % 3]

            def mmdn(e, hid_=hid_, wd_=wd_, st=st):
                for hf, Py in ((0, P4), (1, P7)):
                    for fc in range(4):
                        ins = e.matmul(Py[:, 0:512], lhsT=hid_[:, fc, st * 128:(st + 1) * 128], rhs=wd_[:, fc, hf * 512:(hf + 1) * 512],
                                       start=(fc == 0), stop=(fc == 3))
                return ins
            fw.op("pe", mmdn, reads=[hid_, wd_], writes=[P4, P7])
            fw.op("act", lambda e, y_=y_: e.activation(out=y_[:, 0:512], in_=P4[:, 0:512], func=AF.Copy), reads=[P4], writes=[y_])
            fw.op("dve", lambda e, y_=y_: e.tensor_copy(out=y_[:, 512:1024], in_=P7[:, 0:512]), reads=[P7], writes=[y_])
            r0 = ex * CAP + st * 128
            fw.dma("sp", y_d, y_, lambda e, y_=y_, r0=r0: e.dma_start(out=y_d.t[r0:r0 + 128, :], in_=y_[:]), nowaw=True, lane=yc % 3)

    def step(gen):
        if gen is not None:
            next(gen, None)

    moe_load(0)
    moe_load(1)
    for _ in moe_T(0):
        pass
    for ex in range(NEXP + 1):
        if ex + 2 < NEXP:
            moe_load(ex + 2)
        gu = moe_GU(ex) if ex < NEXP else None
        tr_ = moe_T(ex + 1) if ex + 1 < NEXP else None
        dn = moe_DN(ex - 1) if ex >= 1 else None
        for g_ in (gu, tr_, dn):
            step(g_)
        for k in range(4):
            step(gu)
            if k < 3:
                step(tr_)
                step(dn)
        for g_ in (gu, tr_, dn):
            if g_ is not None:
                for _ in g_:
                    pass

    fw.barrier()
    fw.release(base_mark)

    ln2g = fw.sb("ln2g", [128, D], F32)
    ln2b = fw.sb("ln2b", [128, D], F32)
    fw.dma("sp", ln2g, lnp_d, lambda e: e.dma_start(out=ln2g[:], in_=lnp_d.t[2:3, :].partition_broadcast(128)))
    fw.dma("sp", ln2b, lnp_d, lambda e: e.dma_start(out=ln2b[:], in_=lnp_d.t[3:4, :].partition_broadcast(128)))
    NBF = 8
    Y0 = [fw.sb(f"Y0_{i}", [128, D], F32) for i in range(NBF)]
    Y1 = [fw.sb(f"Y1_{i}", [128, D], F32) for i in range(NBF)]
    hr = [fw.sb(f"hr{i}", [128, D], F32) for i in range(NBF)]
    zz = [fw.sb(f"zz{i}", [128, D], F32) for i in range(NBF)]
    s1f = fw.sb("s1f", [128, 4], F32)
    s2f = fw.sb("s2f", [128, 4], F32)
    junkF = fw.sb("junkF", [128, D], BF16)
    mvf = fw.sb("mvf", [128, 4, 2], F32)
    sdf = fw.sb("sdf", [128, 4], F32)
    rstdf = fw.sb("rstdf", [128, 4], F32)
    nmrf = fw.sb("nmrf", [128, 4], F32)
    epsb2 = fw.sb("epsbb", [128, 1], F32)
    fw.op("dve", lambda e: e.memset(epsb2[:], LN_EPS), writes=[epsb2])

    def fin_load(ti):
        a0, a1, h_ = Y0[ti % NBF], Y1[ti % NBF], hr[ti % NBF]
        fw.dma("pool", a0, y_d, lambda e, a0=a0, ti=ti: e.indirect_dma_start(
            out=a0[:, :], out_offset=None, in_=y_d.t[:, :],
            in_offset=bass.IndirectOffsetOnAxis(ap=dest_all[:, ti, 0:1], axis=0)), extra_reads=[dest_all])
        fw.dma("pool", a1, y_d, lambda e, a1=a1, ti=ti: e.indirect_dma_start(
            out=a1[:, :], out_offset=None, in_=y_d.t[:, :],
            in_offset=bass.IndirectOffsetOnAxis(ap=dest_all[:, ti, 1:2], axis=0)), extra_reads=[dest_all])
        fw.dma("sp", h_, h_d, lambda e, h_=h_, ti=ti: e.dma_start(out=h_[:], in_=h_d.t[ti * 128:(ti + 1) * 128, :]))

    for ti in range(4):
        fin_load(ti)
    for grp in range(8):
        tis = list(range(grp * 4, grp * 4 + 4))
        if grp + 1 < 8:
            for ti in range(grp * 4 + 4, grp * 4 + 8):
                fin_load(ti)
        finP = [([P01], P01[:, 0:512], P01[:, 512:1024]), ([P23], P23[:, 0:512], P23[:, 512:1024]),
                ([P4, P5], P4[:, 0:512], P5[:, 0:512]), ([P6, P7], P6[:, 0:512], P7[:, 0:512])]
        for k, ti in enumerate(tis):
            a0 = Y0[ti % NBF]
            Ts, lo_ap, hi_ap = finP[k]
            for hf, ap in ((0, lo_ap), (1, hi_ap)):
                fw.op("act", lambda e, a0=a0, ap=ap, hf=hf, ti=ti: e.activation(out=ap, in_=a0[:, hf * 512:(hf + 1) * 512], func=AF.Identity,
                                                                              scale=gate_all[:, ti, 0:1]), reads=[a0, gate_all], writes=Ts)
        for k, ti in enumerate(tis):
            a1, h_, z_ = Y1[ti % NBF], hr[ti % NBF], zz[ti % NBF]
            Ts, lo_ap, hi_ap = finP[k]
            for hf, ap in ((0, lo_ap), (1, hi_ap)):
                fw.op("dve", lambda e, a1=a1, ap=ap, hf=hf, ti=ti: e.scalar_tensor_tensor(
                    out=ap, in0=a1[:, hf * 512:(hf + 1) * 512], scalar=gate_all[:, ti, 1:2], in1=ap,
                    op0=ALU.mult, op1=ALU.add), reads=[a1, gate_all] + Ts, writes=Ts)
            for hf, ap in ((0, lo_ap), (1, hi_ap)):
                fw.op("dve", lambda e, h_=h_, z_=z_, ap=ap, hf=hf: e.scalar_tensor_tensor(
                    out=z_[:, hf * 512:(hf + 1) * 512], in0=h_[:, hf * 512:(hf + 1) * 512], scalar=ALPHA, in1=ap,
                    op0=ALU.mult, op1=ALU.add), reads=[h_] + Ts, writes=[z_])
        for k, ti in enumerate(tis):
            z_ = zz[ti % NBF]
            fw.op("act", lambda e, z_=z_, k=k: e.activation(out=junkF[:], in_=z_[:], func=AF.Identity, accum_out=s1f[:, k:k + 1]),
                  reads=[z_], writes=[s1f, junkF])
            fw.op("act", lambda e, z_=z_, k=k: e.activation(out=junkF[:], in_=z_[:], func=AF.Square, accum_out=s2f[:, k:k + 1]),
                  reads=[z_], writes=[s2f, junkF])
        fw.op("dve", lambda e: e.tensor_scalar(out=mvf[:, :, 0], in0=s1f[:], scalar1=1.0 / D, scalar2=None, op0=ALU.mult), reads=[s1f], writes=[mvf])
        fw.op("dve", lambda e: e.tensor_tensor(out=mvf[:, :, 1], in0=mvf[:, :, 0], in1=mvf[:, :, 0], op=ALU.mult), reads=[mvf], writes=[mvf])
        fw.op("dve", lambda e: e.scalar_tensor_tensor(out=mvf[:, :, 1], in0=s2f[:], scalar=1.0 / D, in1=mvf[:, :, 1], op0=ALU.mult, op1=ALU.subtract),
              reads=[s2f, mvf], writes=[mvf])
        fw.op("act", lambda e: e.activation(out=sdf[:], in_=mvf[:, :, 1], func=AF.Sqrt, bias=epsb2[:, 0:1], scale=1.0), reads=[mvf, epsb2], writes=[sdf])
        fw.op("dve", lambda e: e.reciprocal(out=rstdf[:], in_=sdf[:]), reads=[sdf], writes=[rstdf])
        fw.op("dve", lambda e: e.scalar_tensor_tensor(out=nmrf[:], in0=mvf[:, :, 0], scalar=-1.0, in1=rstdf[:], op0=ALU.mult, op1=ALU.mult),
              reads=[mvf, rstdf], writes=[nmrf])
        for k, ti in enumerate(tis):
            z_ = zz[ti % NBF]
            Ts, lo_ap, hi_ap = finP[k]
            for hf, ap in ((0, lo_ap), (1, hi_ap)):
                fw.op("act", lambda e, z_=z_, k=k, ap=ap, hf=hf: e.activation(out=ap, in_=z_[:, hf * 512:(hf + 1) * 512], func=AF.Identity,
                                                                             bias=nmrf[:, k:k + 1], scale=rstdf[:, k:k + 1]),
                      reads=[z_, nmrf, rstdf], writes=Ts)
        for k, ti in enumerate(tis):
            z_ = zz[ti % NBF]
            Ts, lo_ap, hi_ap = finP[k]
            for hf, ap in ((0, lo_ap), (1, hi_ap)):
                fw.op("dve", lambda e, z_=z_, ap=ap, hf=hf: e.tensor_tensor(out=z_[:, hf * 512:(hf + 1) * 512], in0=ap,
                                                                         in1=ln2g[:, hf * 512:(hf + 1) * 512], op=ALU.mult),
                      reads=Ts + [ln2g], writes=[z_])
            fw.op("pool", lambda e, z_=z_: e.tensor_tensor(out=z_[:], in0=z_[:], in1=ln2b[:], op=ALU.add), reads=[z_, ln2b], writes=[z_])
            bb, tq = ti // 16, ti % 16
            fw.dma("sp", out_d, z_, lambda e, z_=z_, bb=bb, tq=tq: e.dma_start(out=out_d.t[bb, tq * 128:(tq + 1) * 128, :], in_=z_[:]),
                   nowaw=True, lane=ti % NBF)
    fw.wait_all("sp", [out_d])
    fw.finish()
    return nc


def _rope_tables():
    half = 8
    inv_freq = (np.float32(500000.0) ** (-(np.arange(half, dtype=np.float32) / np.float32(half)))).astype(np.float32)
    ang = (np.arange(S, dtype=np.float32)[:, None] * inv_freq[None, :]).astype(np.float32)
    return np.stack([np.cos(ang), np.sin(ang)]).astype(np.float32)


def _consts():
    c = np.zeros((128, 288), np.float32)
    c[:, 0:128] = np.eye(128, dtype=np.float32)
    c[:, 128:256] = np.triu(np.ones((128, 128), np.float32), k=1)
    c[:, 256:288] = (np.arange(32, dtype=np.float32) * CAP)[None, :]
    return c


def prep_inputs(x, w_in, gate_bias, w_attn_up, w_conv_up, conv_w, w_out, ln1_g, ln1_b,
                router_group_w, router_group_b, router_expert_w, router_expert_b,
                w_gate_e, w_up_e, w_down_e, ln2_g, ln2_b):
    f = lambda a: np.ascontiguousarray(np.asarray(a, dtype=np.float32))
    w = f(w_in)[0]
    w_tm = np.concatenate([w[:, 0:512], w[:, 768:1280], w[:, 512:640], w[:, 1280:1344], w[:, 640:768], w[:, 1344:1352]], axis=1)
    w_fm = w[:, 1352:4936]
    shared = {
        "w_tm": f(w_tm), "w_fm": f(w_fm), "cs": _rope_tables(), "consts": _consts(),
        "gbias": f(f(gate_bias)[0].reshape(16, 128).T),
        "convw": f(f(conv_w)[0].T.reshape(4, 128, 3).transpose(1, 0, 2)),
        "w_aup": f(w_attn_up)[0], "w_cup": f(w_conv_up)[0], "w_out": f(w_out)[0],
        "lnp": f(np.stack([f(ln1_g)[0], f(ln1_b)[0], f(ln2_g)[0], f(ln2_b)[0]])),
        "w_r": f(np.concatenate([f(router_group_w)[0], f(router_expert_w)[0].transpose(1, 0, 2).reshape(D, 32)], axis=1)),
        "b_r": f(np.concatenate([f(router_group_b)[0], f(router_expert_b)[0].reshape(32)])[None, :]),
        "w_gate": f(w_gate_e)[0], "w_up": f(w_up_e)[0], "w_down": f(w_down_e)[0],
        "zeros": np.zeros((128, D), dtype=ml_dtypes.bfloat16),
    }
    xx = f(x)
    return [dict(shared, x=np.ascontiguousarray(xx[2 * i:2 * i + 2])) for i in range(NCORES)]


def kernel(**inputs):
    in_maps = prep_inputs(**inputs)
    nc = build_program()
    res = run_bass_kernel_spmd(nc, in_maps, core_ids=list(range(NCORES)))
    return np.concatenate([np.asarray(r["out"], dtype=np.float32) for r in res.results], axis=0)
```
